# Optimizing a Trainium2 kernel written in Bass

```python
import math
import jax
import jax.numpy as jnp
from jax import lax
import numpy as np

D_MODEL = 1024
BATCH = 16
SEQ = 2048
DEPTH = 2

N_MIXERS = 2
N_ATTN_LAYERS = (DEPTH + N_MIXERS - 1) // N_MIXERS
N_MLSTM_LAYERS = DEPTH // N_MIXERS
DA_HEADS = 8
DA_HEAD_DIM = 64
DA_WIDTH = DA_HEADS * 2 * DA_HEAD_DIM
DA_QBLOCK = 128
N_BUCKETS = 32
MAX_DISTANCE = 128
ML_INNER = 2 * D_MODEL
ML_HEADS = 4
ML_HEAD_DIM = ML_INNER // ML_HEADS
ML_CONV = 5
ML_QKV_BLOCK = 4
ML_CHUNK = 128
N_GROUPS = 4
EXPERTS_PER_GROUP = 8
N_EXPERTS = N_GROUPS * EXPERTS_PER_GROUP
TOP_K_IN_GROUP = 2
D_EXPERT = D_MODEL // 4
EPS = 1e-6

kernel_name = 'hybrid_diffattn_mlstm_hmoe_encoder'


def rmsnorm(x, g):
    xf = x.astype(jnp.float32)
    y = xf * lax.rsqrt(jnp.mean(xf * xf, axis=-1, keepdims=True) + EPS)
    return (y * g.astype(jnp.float32)).astype(x.dtype)


def t5_bucket(rel):
    nb = N_BUCKETS // 2
    max_exact = nb // 2
    ret = jnp.where(rel > 0, nb, 0)
    n = jnp.abs(rel)
    nf = jnp.maximum(n, 1).astype(jnp.float32)
    large = max_exact + (jnp.log(nf / max_exact) / math.log(MAX_DISTANCE / max_exact)
                         * (nb - max_exact)).astype(jnp.int32)
    large = jnp.minimum(large, nb - 1)
    return ret + jnp.where(n < max_exact, n, large)


def diff_attention(h, w_in, w_out, q_gain, k_gain, lam_q1, lam_k1, lam_q2, lam_k2, subln_g, rel_table, layer_idx):
    B, S, _ = h.shape
    H, dh = DA_HEADS, DA_HEAD_DIM
    q, k, v = jnp.split(h @ w_in, 3, axis=-1)
    q = rmsnorm(q.reshape(B, S, H, 2, dh), q_gain)
    k = rmsnorm(k.reshape(B, S, H, 2, dh), k_gain)
    q = q.transpose(3, 0, 2, 1, 4) * dh ** -0.5
    k = k.transpose(3, 0, 2, 1, 4)
    v = v.reshape(B, S, H, 2 * dh).transpose(0, 2, 1, 3)
    lam_init = 0.8 - 0.6 * math.exp(-0.3 * layer_idx)
    lam = (jnp.exp(jnp.sum(lam_q1 * lam_k1).astype(jnp.float32))
           - jnp.exp(jnp.sum(lam_q2 * lam_k2).astype(jnp.float32)) + lam_init)
    kpos = jnp.arange(S)

    def block(i):
        start = i * DA_QBLOCK
        qb = lax.dynamic_slice_in_dim(q, start, DA_QBLOCK, axis=3)
        qpos = start + jnp.arange(DA_QBLOCK)
        bias = rel_table[t5_bucket(kpos[None, :] - qpos[:, None])]
        bias = bias.transpose(2, 0, 1).astype(jnp.float32)
        s = jnp.einsum('mbhqd,mbhkd->mbhqk', qb, k).astype(jnp.float32) + bias
        a = jax.nn.softmax(s, axis=-1)
        attn = a[0] - lam * a[1]
        return jnp.einsum('bhqk,bhkd->bhqd', attn.astype(v.dtype), v)

    o = lax.map(block, jnp.arange(S // DA_QBLOCK))
    o = o.transpose(1, 0, 3, 2, 4).reshape(B, S, H, 2 * dh)
    o = rmsnorm(o, subln_g) * (1.0 - lam_init)
    return o.reshape(B, S, DA_WIDTH) @ w_out


def conv_centred(x, w, b):
    pad = w.shape[0] // 2
    y = lax.conv_general_dilated(x, w[:, None, :], window_strides=(1,), padding=[(pad, pad)],
                                 dimension_numbers=('NWC', 'WIO', 'NWC'),
                                 feature_group_count=x.shape[-1])
    return y + b


def blockdiag(x, w):
    B, S, C = x.shape
    nb, blk, _ = w.shape
    return jnp.einsum('bsni,nio->bsno', x.reshape(B, S, nb, blk), w).reshape(B, S, C)


def mlstm_scan(q, k, v, i_pre, f_pre):
    B, S, H, dk = q.shape
    dv = v.shape[-1]
    L = ML_CHUNK
    NC = S // L
    f32 = jnp.float32

    def to_chunks(t):
        t = t.reshape((B, NC, L, H) + t.shape[3:])
        return jnp.moveaxis(t, (1, 3), (0, 2))

    qc = to_chunks(q.astype(f32))
    kc = to_chunks(k.astype(f32) * dk ** -0.5)
    vc = to_chunks(v.astype(f32))
    lfc = to_chunks(jax.nn.log_sigmoid(f_pre.astype(f32)))
    igc = to_chunks(i_pre.astype(f32))
    causal = jnp.tril(jnp.ones((L, L), dtype=bool))

    def step(carry, xs):
        C, n, m = carry
        qb, kb, vb, lf, ii = xs
        b = jnp.cumsum(lf, axis=-1)
        dmat = jnp.where(causal, b[..., :, None] - b[..., None, :] + ii[..., None, :], -jnp.inf)
        inter = b + m[..., None]
        m_t = jnp.maximum(inter, jnp.max(dmat, axis=-1))
        w_intra = jnp.exp(dmat - m_t[..., None])
        w_inter = jnp.exp(inter - m_t)
        s = jnp.einsum('bhtd,bhsd->bhts', qb, kb) * w_intra
        num = (w_inter[..., None] * jnp.einsum('bhtk,bhkv->bhtv', qb, C)
               + jnp.einsum('bhts,bhsv->bhtv', s, vb))
        den = w_inter * jnp.einsum('bhtk,bhk->bht', qb, n) + jnp.sum(s, axis=-1)
        h = num / jnp.maximum(jnp.abs(den), jnp.exp(-m_t))[..., None]
        bL = b[..., -1]
        g = bL[..., None] - b + ii
        m_new = jnp.maximum(bL + m, jnp.max(g, axis=-1))
        decay = jnp.exp(bL + m - m_new)
        kw = kb * jnp.exp(g - m_new[..., None])[..., None]
        C_new = decay[..., None, None] * C + jnp.einsum('bhsk,bhsv->bhkv', kw, vb)
        n_new = decay[..., None] * n + jnp.sum(kw, axis=2)
        return (C_new, n_new, m_new), h

    init = (jnp.zeros((B, H, dk, dv), f32), jnp.zeros((B, H, dk), f32), jnp.zeros((B, H), f32))
    _, hs = lax.scan(step, init, (qc, kc, vc, lfc, igc))
    return hs.transpose(1, 0, 3, 2, 4).reshape(B, S, H, dv)


def mlstm_mixer(h, w_in, conv_w, conv_b, wq, wk, wv, gate_w, gate_b, outnorm_g, skip, w_out):
    B, S, _ = h.shape
    H, dv = ML_HEADS, ML_HEAD_DIM
    xm, z = jnp.split(h @ w_in, 2, axis=-1)
    xc = jax.nn.silu(conv_centred(xm, conv_w, conv_b))
    q = blockdiag(xc, wq)
    k = blockdiag(xc, wk)
    v = blockdiag(xm, wv)
    pre = (jnp.einsum('bsc,dcg->dbsg', q, gate_w[:, 0]) + jnp.einsum('bsc,dcg->dbsg', k, gate_w[:, 1])
           + jnp.einsum('bsc,dcg->dbsg', v, gate_w[:, 2]) + gate_b[:, None, None, :])
    heads = lambda t: t.reshape(B, S, H, dv)
    flip = lambda t: jnp.flip(t, axis=1)
    qh, kh, vh = heads(q), heads(k), heads(v)
    h_fwd = mlstm_scan(qh, kh, vh, pre[0, ..., :H], pre[0, ..., H:])
    h_bwd = flip(mlstm_scan(flip(qh), flip(kh), flip(vh), flip(pre[1, ..., :H]), flip(pre[1, ..., H:])))
    hh = rmsnorm((h_fwd + h_bwd).astype(h.dtype), outnorm_g)
    y = (hh.reshape(B, S, ML_INNER) + skip * xc) * jax.nn.silu(z)
    return y @ w_out


def hier_moe(h, w_group, w_router, w1, w3, w2):
    B, S, D = h.shape
    t = h.reshape(B * S, D)
    g_logits = (t @ w_group).astype(jnp.float32)
    g_prob = jax.nn.softmax(g_logits, axis=-1)
    g_sel = jnp.argmax(g_logits, axis=-1)
    p_g = jnp.take_along_axis(g_prob, g_sel[:, None], axis=-1)
    e_logits = (t @ w_router).astype(jnp.float32).reshape(-1, N_GROUPS, EXPERTS_PER_GROUP)
    e_logits = jnp.take_along_axis(e_logits, g_sel[:, None, None], axis=1)[:, 0]
    top_p, top_i = lax.top_k(jax.nn.softmax(e_logits, axis=-1), TOP_K_IN_GROUP)
    weights = p_g * top_p / jnp.sum(top_p, axis=-1, keepdims=True)
    expert_id = g_sel[:, None] * EXPERTS_PER_GROUP + top_i
    gates = jnp.sum(jax.nn.one_hot(expert_id, N_EXPERTS, dtype=jnp.float32) * weights[..., None], axis=1)

    def expert(acc, xs):
        a, b_, c_, gcol = xs
        y = (jax.nn.silu(t @ a) * (t @ b_)) @ c_
        return acc + gcol[:, None].astype(t.dtype) * y, None

    y, _ = lax.scan(expert, jnp.zeros_like(t), (w1, w3, w2, gates.T))
    return y.reshape(B, S, D)


def setup_inputs(seed: int = 0) -> dict:
    key = jax.random.key(seed)
    ks = iter(jax.random.split(key, 48))
    nrm = lambda shape, scale: scale * jax.random.normal(next(ks), shape, jnp.float32)
    NA, NM = N_ATTN_LAYERS, N_MLSTM_LAYERS
    H, dh = DA_HEADS, DA_HEAD_DIM
    MH, dv = ML_HEADS, ML_HEAD_DIM
    nblk = ML_INNER // ML_QKV_BLOCK
    f_bias = jnp.linspace(3.0, 6.0, MH, dtype=jnp.float32)
    ml_gate_b = jnp.concatenate([nrm((NM, 2, MH), 0.1), f_bias + nrm((NM, 2, MH), 0.1)], axis=-1)
    return {
        'x': nrm((BATCH, SEQ, D_MODEL), 1.0),
        'c': nrm((BATCH, D_MODEL), 1.0),
        'rel_table': nrm((N_BUCKETS, H), 0.5),
        'ada_w': nrm((DEPTH, D_MODEL, 6 * D_MODEL), 0.5 * D_MODEL ** -0.5),
        'ada_b': nrm((DEPTH, 6 * D_MODEL), 0.02),
        'norm_mix_g': 1.0 + nrm((DEPTH, D_MODEL), 0.02),
        'norm_ffn_g': 1.0 + nrm((DEPTH, D_MODEL), 0.02),
        'da_w_in': nrm((NA, D_MODEL, 3 * DA_WIDTH), D_MODEL ** -0.5),
        'da_w_out': nrm((NA, DA_WIDTH, D_MODEL), DA_WIDTH ** -0.5),
        'da_q_gain': 1.0 + nrm((NA, dh), 0.02),
        'da_k_gain': 1.0 + nrm((NA, dh), 0.02),
        'da_lam_q1': nrm((NA, dh), 0.1),
        'da_lam_k1': nrm((NA, dh), 0.1),
        'da_lam_q2': nrm((NA, dh), 0.1),
        'da_lam_k2': nrm((NA, dh), 0.1),
        'da_subln_g': 1.0 + nrm((NA, 2 * dh), 0.02),
        'ml_w_in': nrm((NM, D_MODEL, 2 * ML_INNER), D_MODEL ** -0.5),
        'ml_conv_w': nrm((NM, ML_CONV, ML_INNER), ML_CONV ** -0.5),
        'ml_conv_b': nrm((NM, ML_INNER), 0.01),
        'ml_wq': nrm((NM, nblk, ML_QKV_BLOCK, ML_QKV_BLOCK), ML_QKV_BLOCK ** -0.5),
        'ml_wk': nrm((NM, nblk, ML_QKV_BLOCK, ML_QKV_BLOCK), ML_QKV_BLOCK ** -0.5),
        'ml_wv': nrm((NM, nblk, ML_QKV_BLOCK, ML_QKV_BLOCK), ML_QKV_BLOCK ** -0.5),
        'ml_gate_w': nrm((NM, 2, 3, ML_INNER, 2 * MH), (3 * ML_INNER) ** -0.5),
        'ml_gate_b': ml_gate_b,
        'ml_outnorm_g': 1.0 + nrm((NM, MH, dv), 0.02),
        'ml_skip': 1.0 + nrm((NM, ML_INNER), 0.02),
        'ml_w_out': nrm((NM, ML_INNER, D_MODEL), ML_INNER ** -0.5),
        'moe_w_group': nrm((DEPTH, D_MODEL, N_GROUPS), D_MODEL ** -0.5),
        'moe_w_router': nrm((DEPTH, D_MODEL, N_EXPERTS), D_MODEL ** -0.5),
        'moe_w1': nrm((DEPTH, N_EXPERTS, D_MODEL, D_EXPERT), D_MODEL ** -0.5),
        'moe_w3': nrm((DEPTH, N_EXPERTS, D_MODEL, D_EXPERT), D_MODEL ** -0.5),
        'moe_w2': nrm((DEPTH, N_EXPERTS, D_EXPERT, D_MODEL), D_EXPERT ** -0.5),
    }


def reference(x, c, rel_table, ada_w, ada_b, norm_mix_g, norm_ffn_g,
              da_w_in, da_w_out, da_q_gain, da_k_gain, da_lam_q1, da_lam_k1, da_lam_q2, da_lam_k2, da_subln_g,
              ml_w_in, ml_conv_w, ml_conv_b, ml_wq, ml_wk, ml_wv, ml_gate_w, ml_gate_b, ml_outnorm_g, ml_skip, ml_w_out,
              moe_w_group, moe_w_router, moe_w1, moe_w3, moe_w2):
    c_act = jax.nn.silu(c)
    for i in range(DEPTH):
        mod = (c_act @ ada_w[i] + ada_b[i])[:, None, :]
        sh1, sc1, g1, sh2, sc2, g2 = jnp.split(mod, 6, axis=-1)
        hm = rmsnorm(x, norm_mix_g[i]) * (1.0 + sc1) + sh1
        j = i // N_MIXERS
        if i % N_MIXERS == 0:
            y = diff_attention(hm, da_w_in[j], da_w_out[j], da_q_gain[j], da_k_gain[j],
                               da_lam_q1[j], da_lam_k1[j], da_lam_q2[j], da_lam_k2[j], da_subln_g[j],
                               rel_table, i)
        else:
            y = mlstm_mixer(hm, ml_w_in[j], ml_conv_w[j], ml_conv_b[j], ml_wq[j], ml_wk[j], ml_wv[j],
                            ml_gate_w[j], ml_gate_b[j], ml_outnorm_g[j], ml_skip[j], ml_w_out[j])
        x = x + g1 * y
        hf = rmsnorm(x, norm_ffn_g[i]) * (1.0 + sc2) + sh2
        x = x + g2 * hier_moe(hf, moe_w_group[i], moe_w_router[i], moe_w1[i], moe_w3[i], moe_w2[i])
    return x
```

```python
import numpy as np
from contextlib import ExitStack
import concourse.bass as bass
import concourse.mybir as mybir

F32 = mybir.dt.float32
BF16 = mybir.dt.bfloat16
ALU = mybir.AluOpType
AF = mybir.ActivationFunctionType
AX = mybir.AxisListType


class Buf:
    __slots__ = ("name", "w", "r")

    def __init__(self, name):
        self.name = name
        self.w = None
        self.r = {}


class V:
    __slots__ = ("ap", "bufs")

    def __init__(self, ap, bufs):
        self.ap = ap
        self.bufs = bufs

    def __getitem__(self, idx):
        return V(self.ap[idx], self.bufs)


class T:
    def __init__(self, handle, name, buf=None):
        self.h = handle
        self.buf = buf if buf is not None else Buf(name)

    def __getitem__(self, idx):
        return V(self.h[idx], (self.buf,))

    def v(self, ap):
        return V(ap, (self.buf,))


class Mach:
    NRING = {"sp": 12, "act": 4, "pool": 8}

    def __init__(self, nc, es):
        self.nc = nc
        self.es = es
        self.engs = {"pe": nc.tensor, "act": nc.scalar, "dve": nc.vector,
                     "pool": nc.gpsimd, "sp": nc.sync}
        self.sem = {}
        self.cnt = {}
        for e in ("pe", "act", "dve", "pool"):
            self.sem[e] = es.enter_context(nc.semaphore("c_" + e))
            self.cnt[e] = 0
        self.ring = {}
        self.ring_pos = {}
        for q, n in self.NRING.items():
            self.ring[q] = []
            for i in range(n):
                k = ("dma", q, i)
                self.sem[k] = es.enter_context(nc.semaphore("d_%s%d" % (q, i)))
                self.cnt[k] = 0
                self.ring[q].append(k)
            self.ring_pos[q] = 0
        self.waited = {e: {} for e in self.engs}
        self.ninst = 0

    def _uniq(self, name):
        self._uid = getattr(self, "_uid", 0) + 1
        return "%s_u%d" % (name, self._uid)

    def sbuf(self, name, shape, dtype, es=None):
        name = self._uniq(name)
        h = (es or self.es).enter_context(self.nc.sbuf_tensor(name, list(shape), dtype))
        return T(h, name)

    def psum(self, name, shape, dtype, es=None):
        name = self._uniq(name)
        h = (es or self.es).enter_context(self.nc.psum_tensor(name, list(shape), dtype))
        return T(h, name)

    def _wait(self, eng, tok):
        key, val = tok
        if self.waited[eng].get(key, 0) >= val:
            return
        self.engs[eng].wait_ge(self.sem[key], val)
        self.waited[eng][key] = val

    def _deps(self, eng, reads, writes):
        toks = []
        for v in reads:
            for b in v.bufs:
                if b.w is not None:
                    toks.append(b.w)
        for v in writes:
            for b in v.bufs:
                if b.w is not None and b.w[0] != eng:
                    toks.append(b.w)
                for k, t in b.r.items():
                    if k != eng:
                        toks.append(t)
        for t in toks:
            self._wait(eng, t)

    def _commit(self, tok, reads, writes):
        for v in reads:
            for b in v.bufs:
                b.r[tok[0]] = tok
        for v in writes:
            for b in v.bufs:
                b.w = tok
                b.r = {}

    def emit(self, eng, fn, reads, writes):
        self._deps(eng, reads, writes)
        inst = fn()
        self.cnt[eng] += 1
        inst.then_inc(self.sem[eng], 1)
        tok = (eng, self.cnt[eng])
        self._commit(tok, reads, writes)
        self.ninst += 1
        return tok

    def dma(self, q, out, in_, **kw):
        ring = self.ring[q]
        key = ring[self.ring_pos[q] % len(ring)]
        self.ring_pos[q] += 1
        if self.cnt[key] > 0:
            self._wait(q, (key, self.cnt[key]))
        self._deps(q, [in_], [out])
        inst = self.engs[q].dma_start(out=out.ap, in_=in_.ap, **kw)
        self.cnt[key] += 16
        inst.then_inc(self.sem[key], 16)
        tok = (key, self.cnt[key])
        self._commit(tok, [in_], [out])
        self.ninst += 1
        return tok

    def barrier(self):
        for e in self.engs:
            for k, c in self.cnt.items():
                if c > 0 and k != e:
                    self._wait(e, (k, c))

    def finish(self):
        for k, c in self.cnt.items():
            if c > 0:
                self._wait("sp", (k, c))

    def mm(self, out, lhsT, rhs, start=True, stop=True, extra_w=()):
        return self.emit("pe", lambda: self.nc.tensor.matmul(out.ap, lhsT.ap, rhs.ap, start=start, stop=stop),
                         [lhsT, rhs], [out])

    def transpose(self, out, in_, ident):
        return self.emit("pe", lambda: self.nc.tensor.transpose(out.ap, in_.ap, ident.ap),
                         [in_, ident], [out])

    def act(self, out, in_, func, bias=None, scale=None, accum_out=None, eng="act"):
        reads = [in_]
        kw = {}
        if bias is not None:
            if isinstance(bias, V):
                reads.append(bias)
                kw["bias"] = bias.ap
            else:
                kw["bias"] = bias
        if scale is not None:
            if isinstance(scale, V):
                reads.append(scale)
                kw["scale"] = scale.ap
            else:
                kw["scale"] = scale
        writes = [out]
        if accum_out is not None:
            writes.append(accum_out)
            kw["accum_out"] = accum_out.ap
        return self.emit("act", lambda: self.nc.scalar.activation(out.ap, in_.ap, func, **kw), reads, writes)

    def tt(self, eng, out, in0, in1, op):
        e = self.engs[eng]
        return self.emit(eng, lambda: e.tensor_tensor(out.ap, in0.ap, in1.ap, op), [in0, in1], [out])

    def ts(self, eng, out, in0, s1, s2, op0, op1=None, accum_out=None):
        e = self.engs[eng]
        reads = [in0]
        a1 = s1
        a2 = s2
        if isinstance(s1, V):
            reads.append(s1)
            a1 = s1.ap
        if isinstance(s2, V):
            reads.append(s2)
            a2 = s2.ap
        writes = [out]
        kw = {}
        if op1 is not None:
            kw["op1"] = op1
        if accum_out is not None:
            writes.append(accum_out)
            kw["accum_out"] = accum_out.ap
        return self.emit(eng, lambda: e.tensor_scalar(out.ap, in0.ap, a1, a2, op0, **kw), reads, writes)

    def stt(self, eng, out, in0, scalar, in1, op0, op1):
        e = self.engs[eng]
        reads = [in0, in1]
        a = scalar
        if isinstance(scalar, V):
            reads.append(scalar)
            a = scalar.ap
        return self.emit(eng, lambda: e.scalar_tensor_tensor(out.ap, in0.ap, a, in1.ap, op0, op1), reads, [out])

    def copy(self, eng, out, in_):
        if eng == "act":
            return self.emit("act", lambda: self.nc.scalar.copy(out.ap, in_.ap), [in_], [out])
        e = self.engs[eng]
        return self.emit(eng, lambda: e.tensor_copy(out.ap, in_.ap), [in_], [out])

    def reduce(self, eng, out, in_, op, axis=AX.X):
        e = self.engs[eng]
        return self.emit(eng, lambda: e.tensor_reduce(out.ap, in_.ap, axis, op), [in_], [out])

    def memset(self, eng, out, val):
        e = self.engs[eng]
        return self.emit(eng, lambda: e.memset(out.ap, val), [], [out])

    def recip(self, out, in_):
        return self.emit("dve", lambda: self.nc.vector.reciprocal(out.ap, in_.ap), [in_], [out])


import math
from concourse.bass_utils import run_bass_kernel_spmd
import ml_dtypes

NB = 2
S = 2048
D = 1024
KC = 8
NT = 16
NBLK = 4
EPS = 1e-6
LAM_INIT0 = 0.8 - 0.6 * math.exp(-0.3 * 0)


def _t5_bucket(rel):
    nb = 16
    me = 8
    ret = np.where(rel > 0, nb, 0)
    n = np.abs(rel)
    nf = np.maximum(n, 1).astype(np.float32)
    large = me + (np.log(nf / np.float32(me)) / np.float32(math.log(128 / me)) * np.float32(nb - me)).astype(np.int32)
    large = np.minimum(large, nb - 1)
    return ret + np.where(n < me, n, large)


class K:
    def __init__(self, dbg=None):
        self.dbg = dbg
        nc = bass.Bass("TRN2", target_bir_lowering=False)
        self.nc = nc
        self.es = ExitStack()
        self.m = Mach(nc, self.es)
        self.din = {}

    def dram_in(self, name, shape, dtype=F32):
        h = self.nc.dram_tensor(name, list(shape), dtype, kind="ExternalInput")
        t = T(h.ap(), name)
        self.din[name] = t
        return t

    def dram_out(self, name, shape, dtype=F32):
        h = self.nc.dram_tensor(name, list(shape), dtype, kind="ExternalOutput")
        return T(h.ap(), name)

    def dram_tmp(self, name, shape, dtype=F32):
        h = self.nc.dram_tensor(name, list(shape), dtype, kind="Internal")
        return T(h.ap(), name)

    def declare(self, stages):
        di = self.dram_in
        self.x_d = di("x", [NB, S, D])
        self.cT_d = di("cT", [128, KC, NB])
        self.ada_w_d = di("ada_w", [2, D, 6 * D])
        self.ada_b_d = di("ada_bT", [128, 2, 48])
        self.normg_d = di("normgT", [128, 2, 2, KC])
        self.consts_d = di("consts", [128, 6, 128])
        self.da_w_in_d = di("da_w_in", [D, 3 * D])
        self.da_w_out_d = di("da_w_out", [D, D])
        self.da_vec_d = di("da_vec", [128, 4])
        self.da_lam_d = di("da_lam", [128, 4, 64])
        self.da_G_d = di("da_G", [8, 128, 1152])
        self.da_cb_d = di("da_cb", [128, 8, 2])
        if "moe0" in stages or "moe1" in stages:
            self.moe_wr_d = di("moe_wr", [2, D, 36])
            self.moe_w1_d = di("moe_w1", [2, 32, D, 256])
            self.moe_w3_d = di("moe_w3", [2, 32, D, 256])
            self.moe_w2_d = di("moe_w2", [2, 32, 256, D])
        if "mlstm" in stages:
            self.ml_w_in_d = di("ml_w_in", [D, 4 * D])
            self.ml_w_out_d = di("ml_w_out", [2 * D, D])
            self.ml_vec_d = di("ml_vec", [128, 16, 8])
            self.ml_bd_d = di("ml_bd", [128, 3, 16, 128])
            self.ml_gw_d = di("ml_gw", [128, 3, 16, 16])
            self.ml_gb_d = di("ml_gb", [128, 16])
            self.ml_xc_d = self.dram_tmp("ml_xc_scr", [16, 128, S], BF16)
            self.ml_xm_d = self.dram_tmp("ml_xm_scr", [16, 128, S], BF16)
            self.ml_zs_d = self.dram_tmp("ml_zs_scr", [16, 128, S], BF16)
        self.out_d = self.dram_out("out", [NB, S, D])
        self.gT_d = self.dram_tmp("gT_scr", [32, S])

    def dump(self, tag, view, shape, dtype):
        if not self.dbg or tag not in self.dbg:
            return
        if not hasattr(self, "_dumped"):
            self._dumped = set()
        if tag in self._dumped:
            return
        self._dumped.add(tag)
        h = self.nc.dram_tensor("dbg_" + tag, list(shape), dtype, kind="ExternalOutput")
        t = T(h.ap(), "dbg_" + tag)
        self.m.dma("sp", t[:], view)

    def consts(self):
        m = self.m
        self.cst = m.sbuf("cst", [128, 6, 128], F32)
        m.dma("sp", self.cst[:], self.consts_d[:])
        self.cst_bf = m.sbuf("cst_bf", [128, 6, 128], BF16)
        m.copy("dve", self.cst_bf[:], self.cst[:])
        self.ident = self.cst[:, 0, :]
        self.ones_f = self.cst[:, 1, :]
        self.ident_bf = self.cst_bf[:, 0, :]
        self.ones_bf = self.cst_bf[:, 1, :]
        self.bones_bf = self.cst_bf[:, 2, :]
        self.epsT = m.sbuf("epsT", [128, 1], F32)
        m.memset("dve", self.epsT[:], EPS)

    def prologue(self):
        m = self.m
        nc = self.nc
        mod = m.sbuf("mod", [128, 2, 48, NB], F32)
        self.nscale = m.sbuf("nscale", [128, 2, 2, KC, NB], F32)
        with ExitStack() as es:
            cT = m.sbuf("cT", [128, KC, NB], F32, es)
            cact = m.sbuf("cact", [128, KC, NB], BF16, es)
            adab = m.sbuf("adab", [128, 2, 48], F32, es)
            normg = m.sbuf("normg", [128, 2, 2, KC], F32, es)
            wbuf = [m.sbuf("adaw%d" % i, [128, KC, 768], BF16, es) for i in range(2)]
            ps = m.psum("modps", [128, 2, 48, NB], F32, es)
            m.dma("sp", cT[:], self.cT_d[:])
            m.dma("sp", adab[:], self.ada_b_d[:])
            m.dma("sp", normg[:], self.normg_d[:])
            m.act(cact[:], cT[:], AF.Silu)
            i = 0
            for l in range(2):
                wv = self.ada_w_d.h[l].rearrange("(kc p) n -> p kc n", p=128)
                for g in range(8):
                    wb = wbuf[i % 2]
                    i += 1
                    m.dma("pool", wb[:], self.ada_w_d.v(wv[:, :, g * 768:(g + 1) * 768]))
                    for oc in range(6):
                        c = g * 6 + oc
                        for kc in range(KC):
                            m.mm(ps[:, l, c, :], wb[:, kc, oc * 128:(oc + 1) * 128], cact[:, kc, :],
                                 start=(kc == 0), stop=(kc == KC - 1))
            adab_bc = V(bass.AP(adab.h, 0, [[adab.h[:].ap[0][0], 128], [48, 2], [1, 48], [0, NB]]), (adab.buf,))
            m.tt("dve", mod[:], ps[:], adab_bc, ALU.add)
            self.dump("mod", mod[:], [128, 2, 48, NB], F32)
            self.mod = mod
            for l in range(2):
                for j in range(2):
                    sc = mod[:, l, 8 + 24 * j: 16 + 24 * j, :]
                    gsl = normg.h[:, l, j, :]
                    gbc = V(bass.AP(normg.h, gsl.offset, [list(gsl.ap[0]), [1, KC], [0, NB]]), (normg.buf,))
                    m.stt("dve", self.nscale[:, l, j, :, :], sc, 1.0, gbc, ALU.add, ALU.mult)
            m.barrier()

    def mod_vec(self, l, which, b):
        base = {"sh1": 0, "g1": 16, "sh2": 24, "g2": 40}[which]
        return lambda c: self.mod[:, l, base + c, b:b + 1]

    def load_x(self, b, xT, ps_list):
        m = self.m
        with ExitStack() as es:
            xin = [m.sbuf("xin%d" % i, [128, D], F32, es) for i in range(2)]
            for t in range(NT):
                xi = xin[t % 2]
                m.dma("sp", xi[:], self.x_d[b, t * 128:(t + 1) * 128, :])
                for half in range(2):
                    ps = ps_list[(t * 2 + half) % len(ps_list)]
                    for j in range(4):
                        c = half * 4 + j
                        m.transpose(ps[:, j * 128:(j + 1) * 128], xi[:, c * 128:(c + 1) * 128], self.ident)
                    src = ps.v(ps.h[:, :].rearrange("p (j t) -> p j t", j=4))
                    dst = xT[:, half * 4:(half + 1) * 4, t * 128:(t + 1) * 128]
                    if (t + half) % 2 == 0:
                        m.copy("dve", dst, src)
                    else:
                        m.copy("act", dst, src)
            m.barrier()

    def store_x(self, b, xT, ps_list):
        m = self.m
        with ExitStack() as es:
            xo = [m.sbuf("xo%d" % i, [128, D], F32, es) for i in range(2)]
            for t in range(NT):
                xi = xo[t % 2]
                for half in range(2):
                    ps = ps_list[(t * 2 + half) % len(ps_list)]
                    for j in range(4):
                        c = half * 4 + j
                        m.transpose(ps[:, j * 128:(j + 1) * 128], xT[:, c, t * 128:(t + 1) * 128], self.ident)
                    dst = xi[:, half * 512:(half + 1) * 512]
                    if (t + half) % 2 == 0:
                        m.copy("dve", dst, ps[:, :])
                    else:
                        m.copy("act", dst, ps[:, :])
                m.dma("sp", self.out_d[b, t * 128:(t + 1) * 128, :], xi[:])
            m.barrier()

    def norm_mod(self, xT, hT, l, j, b, psA, psB, es):
        m = self.m
        sq = [m.sbuf("nm_sq%d" % i, [128, KC, 512], BF16, es) for i in range(2)]
        rs = [m.sbuf("nm_rs%d" % i, [128, 512], F32, es) for i in range(2)]
        tmp = [m.sbuf("nm_t%d" % i, [128, 512], F32, es) for i in range(3)]
        shbase = 0 if j == 0 else 24
        ti = 0
        for blk in range(NBLK):
            sl = slice(blk * 512, (blk + 1) * 512)
            s_ = sq[blk % 2]
            ps = (psA, psB)[blk % 2]
            m.act(s_[:], xT[:, :, sl], AF.Square)
            for c in range(KC):
                m.mm(ps[:], self.ones_bf, s_[:, c, :], start=(c == 0), stop=(c == KC - 1))
            r = rs[blk % 2]
            m.act(r[:], ps[:], AF.Ln, bias=self.epsT[:], scale=1.0 / D)
            m.act(r[:], r[:], AF.Exp, scale=-0.5)
            self.dump("nm_r%d" % blk, r[:], [128, 512], F32)
            self.dump("nm_sq%d" % blk, s_[:], [128, KC, 512], BF16)
            for c in range(KC):
                t_ = tmp[ti % 3]
                ti += 1
                m.tt("dve", t_[:], xT[:, c, sl], r[:], ALU.mult)
                m.act(hT[:, c, sl], t_[:], AF.Identity, bias=self.mod[:, l, shbase + c, b:b + 1],
                      scale=self.nscale[:, l, j, c, b:b + 1])

    def attention(self, xT, b):
        m = self.m
        l = 0
        with ExitStack() as es:
            PS = [m.psum("aps%d" % i, [128, 512], F32, es) for i in range(8)]
            pA, pB = PS[0], PS[1]
            sTs = PS[2:4]
            accs = [(PS[4], PS[5]), (PS[6], PS[7])]
            hT = m.sbuf("a_hT", [128, KC, S], BF16, es)
            with ExitStack() as es2:
                self.norm_mod(xT, hT, l, 0, b, pA, pB, es2)
                m.barrier()
            self.dump("a_hT", hT[:], [128, KC, S], BF16)
            dvec0 = m.sbuf("a_vec0", [128, 4], F32, es)
            m.dma("sp", dvec0[:], self.da_vec_d[:])
            dvec = m.sbuf("a_vec", [128, 4], F32, es)
            m.ts("dve", dvec[:, 0:1], dvec0[:, 0:1], 0.125, None, ALU.mult)
            m.copy("dve", dvec[:, 1:2], dvec0[:, 1:2])
            m.ts("dve", dvec[:, 2:3], dvec0[:, 2:3], 1.0 - LAM_INIT0, None, ALU.mult)
            cb = m.sbuf("a_cb", [128, 8, 2], F32, es)
            m.dma("sp", cb[:], self.da_cb_d[:])
            lamv = m.sbuf("a_lamv", [128, 4, 64], F32, es)
            m.dma("sp", lamv[:], self.da_lam_d[:])
            lt = m.sbuf("a_lt", [128, 2, 64], F32, es)
            ls = m.sbuf("a_ls", [128, 2], F32, es)
            le = m.sbuf("a_le", [128, 2], F32, es)
            nlam = m.sbuf("a_nlam", [128, 1], F32, es)
            m.tt("dve", lt[:, 0, :], lamv[:, 0, :], lamv[:, 1, :], ALU.mult)
            m.tt("dve", lt[:, 1, :], lamv[:, 2, :], lamv[:, 3, :], ALU.mult)
            m.reduce("dve", ls[:], lt[:], ALU.add)
            m.act(le[:], ls[:], AF.Exp)
            m.stt("dve", nlam[:], le[:, 1:2], -LAM_INIT0, le[:, 0:1], ALU.add, ALU.subtract)

            wqkv = [m.sbuf("a_w%d" % i, [128, KC, 3, 128], BF16, es) for i in range(2)]
            wout = [m.sbuf("a_wo%d" % i, [128, D], BF16, es) for i in range(2)]
            Gs = [m.sbuf("a_G%d" % i, [128, 1152], F32, es) for i in range(2)]
            qn = [m.sbuf("a_qn%d" % i, [128, 2, S], BF16, es) for i in range(2)]
            for i in range(2):
                m.memset("pool", qn[i][64:128, 0, :], 0.0)
                m.memset("pool", qn[i][0:64, 1, :], 0.0)
            kn = [m.sbuf("a_kn%d" % i, [128, S], BF16, es) for i in range(2)]
            vh = [m.sbuf("a_v%d" % i, [128, NT, 128], BF16, es) for i in range(2)]
            sq = [m.sbuf("a_sq%d" % i, [128, 512], BF16, es) for i in range(2)]
            rs = [m.sbuf("a_rs%d" % i, [128, 512], F32, es) for i in range(2)]
            pT = [m.sbuf("a_pT%d" % i, [128, 512], BF16, es) for i in range(4)]
            rden = [m.sbuf("a_rd%d" % i, [128, 512], F32, es) for i in range(2)]
            t0 = [m.sbuf("a_t0%d" % i, [128, 512], F32, es) for i in range(2)]
            t1 = [m.sbuf("a_t1%d" % i, [128, 512], F32, es) for i in range(2)]
            ob = [m.sbuf("a_o%d" % i, [128, 512], F32, es) for i in range(2)]
            onT = [m.sbuf("a_on%d" % i, [128, 512], BF16, es) for i in range(2)]

            win = self.da_w_in_d.h.rearrange("(kc p) (t h n) -> p kc t h n", p=128, t=3, h=8)
            wo = self.da_w_out_d.h.rearrange("(h p) n -> p h n", p=128)
            cnt = {"pq": 0, "st": 0, "acc": 0, "pt": 0, "o": 0}

            def nxt(k):
                cnt[k] += 1
                return cnt[k] - 1

            def load_head(h):
                for t3 in range(3):
                    m.dma("pool", wqkv[h % 2][:, :, t3, :], self.da_w_in_d.v(win[:, :, t3, h, :]))
                m.dma("pool", wout[h % 2][:], self.da_w_out_d.v(wo[:, h, :]))
                m.dma("sp", Gs[h % 2][:], self.da_G_d[h])

            load_head(0)
            for h in range(8):
                w = wqkv[h % 2]
                wo_h = wout[h % 2]
                G = Gs[h % 2]
                if h + 1 < 8:
                    load_head(h + 1)
                q_h, k_h, v_h = qn[h % 2], kn[h % 2], vh[h % 2]
                items = [(which, blk) for which in (0, 1) for blk in range(NBLK)]

                def projA(ii):
                    which, blk = items[ii]
                    sl = slice(blk * 512, (blk + 1) * 512)
                    pp = PS[ii % 2]
                    for kc in range(KC):
                        m.mm(pp[:], w[:, kc, which, :], hT[:, kc, sl], start=(kc == 0), stop=(kc == KC - 1))
                    m.act(sq[ii % 2][:], pp[:], AF.Square)

                def projB(ii):
                    which, blk = items[ii]
                    sl = slice(blk * 512, (blk + 1) * 512)
                    pp = PS[ii % 2]
                    ssp = PS[2 + (ii % 2)]
                    m.mm(ssp[:], self.bones_bf, sq[ii % 2][:])
                    r = rs[ii % 2]
                    m.act(r[:], ssp[:], AF.Ln, bias=self.epsT[:], scale=1.0 / 64)
                    m.act(r[:], r[:], AF.Exp, scale=-0.5)
                    if which == 1:
                        m.stt("dve", k_h[:, sl], pp[:], dvec[:, 1:2], r[:], ALU.mult, ALU.mult)
                    else:
                        m.stt("dve", q_h[0:64, 0, sl], pp[0:64, :], dvec[0:64, 0:1], r[0:64, :], ALU.mult, ALU.mult)
                        m.stt("dve", q_h[64:128, 1, sl], pp[64:128, :], dvec[64:128, 0:1], r[64:128, :],
                              ALU.mult, ALU.mult)

                for ii in range(len(items) + 1):
                    if ii < len(items):
                        projA(ii)
                    if ii >= 1:
                        projB(ii - 1)
                for g4 in range(4):
                    pp = PS[g4 % 2]
                    for j in range(4):
                        t = g4 * 4 + j
                        for kc in range(KC):
                            m.mm(pp[:, j * 128:(j + 1) * 128], hT[:, kc, t * 128:(t + 1) * 128], w[:, kc, 2, :],
                                 start=(kc == 0), stop=(kc == KC - 1))
                    m.copy("act", v_h[:, g4 * 4:(g4 + 1) * 4, :],
                           pp.v(pp.h[:, :].rearrange("p (j n) -> p j n", j=4)))
                tiles = [(qb, mp, kt) for qb in range(NBLK) for mp in range(2) for kt in range(NT)]
                NTL = len(tiles)
                LA = 2
                deferred = []
                sT3 = PS[1:4]

                def stageA(ti):
                    qb, mp, kt = tiles[ti]
                    rows = slice(mp * 64, (mp + 1) * 64)
                    qsl = slice(qb * 512, (qb + 1) * 512)
                    sT = sT3[ti % 3]
                    m.mm(sT[:], k_h[:, kt * 128:(kt + 1) * 128], q_h[:, mp, qsl])
                    p_ = pT[ti % 4]
                    dk = kt - 4 * qb
                    if dk <= -2:
                        m.act(p_[:], sT[:], AF.Exp, bias=cb[:, h, 0:1])
                    elif dk >= 5:
                        m.act(p_[:], sT[:], AF.Exp, bias=cb[:, h, 1:2])
                    else:
                        Dd = 128 * dk
                        m.tt("dve", sT[:], sT[:], G[:, 512 - Dd:1024 - Dd], ALU.add)
                        m.act(p_[:], sT[:], AF.Exp)

                def epilogue(ti, qb, mp, O, den, gi):
                    qsl = slice(qb * 512, (qb + 1) * 512)
                    par = qb % 2
                    rd = rden[mp]
                    m.act(rd[:], den[:], AF.Ln)
                    m.act(rd[:], rd[:], AF.Exp, scale=-1.0)
                    if mp == 0:
                        m.tt("dve", t0[par][:], O[:], rd[:], ALU.mult)
                        return
                    m.tt("dve", t1[par][:], O[:], rd[:], ALU.mult)
                    o_ = ob[par]
                    m.stt("dve", o_[:], t1[par][:], nlam[:], t0[par][:], ALU.mult, ALU.add)
                    if qb == 0:
                        self.dump("a_o", o_[:], [128, 512], F32)
                    s_ = sq[par]
                    m.act(s_[:], o_[:], AF.Square)
                    on_ = onT[par]
                    r = rs[par]

                    def e_ss():
                        m.mm(PS[0][:], self.ones_bf, s_[:])
                        m.act(r[:], PS[0][:], AF.Ln, bias=self.epsT[:], scale=1.0 / 128)
                        m.act(r[:], r[:], AF.Exp, scale=-0.5)
                        m.stt("dve", on_[:], o_[:], dvec[:, 2:3], r[:], ALU.mult, ALU.mult)
                    deferred.append((ti + 3, e_ss))
                    for oc in range(KC):
                        def e_op(oc=oc):
                            m.mm(PS[0][:], wo_h[:, oc * 128:(oc + 1) * 128], on_[:])
                            m.stt("dve", xT[:, oc, qsl], PS[0][:], self.mod[:, l, 16 + oc, b:b + 1], xT[:, oc, qsl],
                                  ALU.mult, ALU.add)
                        deferred.append((ti + 7 + 2 * oc, e_op))

                def stageB(ti):
                    qb, mp, kt = tiles[ti]
                    gi = qb * 2 + mp
                    O, den = accs[gi % 2]
                    p_ = pT[ti % 4]
                    m.mm(O[:], v_h[:, kt, :], p_[:], start=(kt == 0), stop=(kt == NT - 1))
                    m.mm(den[:], self.ones_bf, p_[:], start=(kt == 0), stop=(kt == NT - 1))
                    if kt == NT - 1:
                        epilogue(ti, qb, mp, O, den, gi)

                for step in range(NTL + LA):
                    if step < NTL:
                        stageA(step)
                    if step >= LA:
                        stageB(step - LA)
                    while deferred and deferred[0][0] <= step - LA:
                        deferred.pop(0)[1]()
                while deferred:
                    deferred.pop(0)[1]()
            m.barrier()

    def moe(self, xT, b, l):
        m = self.m
        with ExitStack() as es:
            PS = [m.psum("mps%d" % i, [128, 512], F32, es) for i in range(8)]
            hT = m.sbuf("m_hT", [128, KC, S], BF16, es)
            w1b = [m.sbuf("m_w1%d" % i, [128, KC, 2, 256], BF16, es) for i in range(2)]
            w3b = [m.sbuf("m_w3%d" % i, [128, KC, 2, 256], BF16, es) for i in range(2)]
            w2b = [m.sbuf("m_w2%d" % i, [128, 2, 2, D], BF16, es) for i in range(2)]
            w1v = self.moe_w1_d.h[l].rearrange("e (kc p) n -> p kc e n", p=128)
            w3v = self.moe_w3_d.h[l].rearrange("e (kc p) n -> p kc e n", p=128)
            w2v = self.moe_w2_d.h[l].rearrange("e (hc p) n -> p e hc n", p=128)

            def load_w(ep):
                w1, w3, w2 = w1b[ep % 2], w3b[ep % 2], w2b[ep % 2]
                for e_ in range(2):
                    m.dma("pool", w1[:, :, e_, :], self.moe_w1_d.v(w1v[:, :, 2 * ep + e_, :]))
                    m.dma("pool", w3[:, :, e_, :], self.moe_w3_d.v(w3v[:, :, 2 * ep + e_, :]))
                    m.dma("pool", w2[:, e_, :, :], self.moe_w2_d.v(w2v[:, 2 * ep + e_, :, :]))

            load_w(0)
            with ExitStack() as es2:
                self.norm_mod(xT, hT, l, 1, b, PS[0], PS[1], es2)
                m.barrier()
            with ExitStack() as es2:
                wr = m.sbuf("m_wr", [128, KC, 36], BF16, es2)
                m.dma("pool", wr[:], self.moe_wr_d.v(self.moe_wr_d.h[l].rearrange("(kc p) n -> p kc n", p=128)))
                gT = m.sbuf("m_gT", [32, S], F32, es2)
                BIG = 30000.0

                def sb(name, shape):
                    return m.sbuf(name, shape, F32, es2)
                gl, el, gmx, gsh, gex, gsum, pen, em, m12, msk, em2, esh, ee, dd, e2, den, rr, gates = (
                    sb("r_gl", [128, NT, 4]), sb("r_el", [128, NT, 32]), sb("r_gmx", [128, NT]),
                    sb("r_gsh", [128, NT, 4]), sb("r_gex", [128, NT, 4]), sb("r_gsum", [128, NT]),
                    sb("r_pen", [128, NT, 4]), sb("r_em", [128, NT, 32]), sb("r_m12", [128, 2, NT]),
                    sb("r_msk", [128, NT, 32]), sb("r_em2", [128, NT, 32]), sb("r_esh", [128, NT, 32]),
                    sb("r_ee", [128, NT, 32]), sb("r_dd", [128, NT]), sb("r_e2", [128, NT]),
                    sb("r_den", [128, NT]), sb("r_rr", [128, NT]), sb("r_gates", [128, NT, 32]))

                def bc(t, n):
                    return V(bass.AP(t.h, 0, [[t.h[:].ap[0][0], 128], [1, NT], [0, n]]), (t.buf,))

                for half in range(2):
                    ps = PS[half]
                    for t8 in range(8):
                        t = half * 8 + t8
                        for kc in range(KC):
                            m.mm(ps[:, t8 * 36:(t8 + 1) * 36], hT[:, kc, t * 128:(t + 1) * 128], wr[:, kc, :],
                                 start=(kc == 0), stop=(kc == KC - 1))
                    ps3 = ps.v(ps.h[:, 0:288].rearrange("p (t c) -> p t c", t=8))
                    m.copy("act", gl[:, half * 8:(half + 1) * 8, :], ps3[:, :, 0:4])
                    m.copy("act", el[:, half * 8:(half + 1) * 8, :], ps3[:, :, 4:36])
                m.reduce("dve", gmx[:], gl[:], ALU.max)
                m.tt("dve", gsh[:], gl[:], bc(gmx, 4), ALU.subtract)
                m.act(gex[:], gsh[:], AF.Exp)
                m.reduce("dve", gsum[:], gex[:], ALU.add)
                m.ts("dve", pen[:], gsh[:], 0.0, BIG, ALU.is_ge, ALU.mult)
                m.ts("dve", pen[:], pen[:], -BIG, None, ALU.add)
                pen_bc = V(bass.AP(pen.h, 0, [[pen.h[:].ap[0][0], 128], [1, NT * 4], [0, 8]]), (pen.buf,))
                m.tt("dve", em.v(em.h[:].rearrange("p t (g e) -> p (t g) e", g=4)),
                     el.v(el.h[:].rearrange("p t (g e) -> p (t g) e", g=4)), pen_bc, ALU.add)
                m.reduce("dve", m12[:, 0, :], em[:], ALU.max)
                m.tt("dve", msk[:], em[:], bc(T(m12.h, "x", m12.buf), 32), ALU.is_ge)
                m.stt("dve", em2[:], msk[:], -BIG, em[:], ALU.mult, ALU.add)
                m.reduce("dve", m12[:, 1, :], em2[:], ALU.max)
                m2_bc = V(bass.AP(m12.h, NT, [[m12.h[:].ap[0][0], 128], [1, NT], [0, 32]]), (m12.buf,))
                m.tt("dve", msk[:], em[:], m2_bc, ALU.is_ge)
                m.tt("dve", esh[:], em[:], bc(T(m12.h, "x", m12.buf), 32), ALU.subtract)
                m.act(ee[:], esh[:], AF.Exp)
                m.tt("dve", dd[:], m12[:, 1, :], m12[:, 0, :], ALU.subtract)
                m.act(e2[:], dd[:], AF.Exp)
                m.stt("dve", den[:], e2[:], 1.0, gsum[:], ALU.add, ALU.mult)
                m.recip(rr[:], den[:])
                m.tt("dve", gates[:], ee[:], bc(rr, 32), ALU.mult)
                m.tt("dve", gates[:], gates[:], msk[:], ALU.mult)
                for g4 in range(4):
                    pst = PS[2 + g4 % 2]
                    for j in range(4):
                        t = g4 * 4 + j
                        m.transpose(pst[0:32, j * 128:(j + 1) * 128], gates[:, t, :], self.ident)
                    m.copy("act", gT[:, g4 * 512:(g4 + 1) * 512], pst[0:32, :])
                m.dma("sp", self.gT_d[:], gT[:])
                m.barrier()
            gbc = [m.sbuf("m_gbc%d" % i, [128, 2, 512], F32, es) for i in range(2)]
            sil = [m.sbuf("m_sil%d" % i, [128, 512], F32, es) for i in range(2)]
            tt_ = [m.sbuf("m_tt%d" % i, [128, 512], F32, es) for i in range(2)]
            aT = [m.sbuf("m_aT%d" % i, [128, 4, 512], BF16, es) for i in range(2)]
            gtv = self.gT_d.h
            hp = [PS[0], PS[1], PS[2], PS[3]]
            yp = [PS[4], PS[5], PS[6], PS[7]]
            ci = 0
            yi = 0
            gi = 0
            def load_g(it):
                ep, sbk = it // NBLK, it % NBLK
                src = bass.AP(gtv.tensor, 2 * ep * S + sbk * 512, [[0, 128], [S, 2], [1, 512]])
                m.dma("sp", gbc[it % 2][:], self.gT_d.v(src))

            ctr = {"ci": 0, "yi": 0}

            def Hpart(it):
                ep, sbk = it // NBLK, it % NBLK
                w1, w3 = w1b[ep % 2], w3b[ep % 2]
                sl = slice(sbk * 512, (sbk + 1) * 512)
                g_ = gbc[it % 2]
                a_ = aT[it % 2]
                for hc in range(4):
                    e2_, hh = hc // 2, hc % 2
                    ci = ctr["ci"]
                    h1p, h3p = hp[(ci % 2) * 2], hp[(ci % 2) * 2 + 1]
                    for kc in range(KC):
                        m.mm(h1p[:], w1[:, kc, e2_, hh * 128:(hh + 1) * 128], hT[:, kc, sl],
                             start=(kc == 0), stop=(kc == KC - 1))
                    for kc in range(KC):
                        m.mm(h3p[:], w3[:, kc, e2_, hh * 128:(hh + 1) * 128], hT[:, kc, sl],
                             start=(kc == 0), stop=(kc == KC - 1))
                    s_ = sil[ci % 2]
                    t_ = tt_[ci % 2]
                    ctr["ci"] += 1
                    m.act(s_[:], h1p[:], AF.Silu)
                    m.tt("dve", t_[:], h3p[:], s_[:], ALU.mult)
                    m.tt("pool", a_[:, hc, :], t_[:], g_[:, e2_, :], ALU.mult)

            def Ypart(it):
                ep, sbk = it // NBLK, it % NBLK
                w2 = w2b[ep % 2]
                sl = slice(sbk * 512, (sbk + 1) * 512)
                a_ = aT[it % 2]
                for oc in range(KC):
                    y_ = yp[ctr["yi"] % 4]
                    ctr["yi"] += 1
                    for hc in range(4):
                        m.mm(y_[:], w2[:, hc // 2, hc % 2, oc * 128:(oc + 1) * 128], a_[:, hc, :],
                             start=(hc == 0), stop=(hc == 3))
                    m.stt("dve", xT[:, oc, sl], y_[:], self.mod[:, l, 40 + oc, b:b + 1], xT[:, oc, sl],
                          ALU.mult, ALU.add)

            NIT = 16 * NBLK
            load_g(0)
            load_g(1)
            Hpart(0)
            for it in range(NIT):
                ep, sbk = it // NBLK, it % NBLK
                if sbk == 0 and ep + 1 < 16:
                    load_w(ep + 1)
                if it + 2 < NIT:
                    load_g(it + 2)
                if it + 1 < NIT:
                    Hpart(it + 1)
                Ypart(it)
            m.barrier()

    def mlstm(self, xT, b):
        m = self.m
        nc = self.nc
        l = 1
        ISQ = 512 ** -0.5
        with ExitStack() as es:
            dCps = m.psum("l_dC", [128, 4, 512], F32, es)
            PS4 = m.psum("l_ps4", [128, 512], F32, es)
            PS5 = m.psum("l_ps5", [128, 512], F32, es)
            PS6 = m.psum("l_ps6", [128, 4, 128], BF16, es)
            PS7 = m.psum("l_ps7", [128, 512], F32, es)
            banks = [T(dCps.h, "dCb%d" % j) for j in range(4)]

            def bank(j):
                return banks[j].v(dCps.h[:, j, :])

            lvec = m.sbuf("l_vec", [128, 16, 8], F32, es)
            m.dma("sp", lvec[:], self.ml_vec_d[:])
            gb = m.sbuf("l_gb", [128, 16], F32, es)
            m.dma("sp", gb[:], self.ml_gb_d[:])
            pre = m.sbuf("l_pre", [128, 16, 2, 8], F32, es)
            g_all = m.sbuf("l_g", [128, 16, 2, 4], F32, es)
            dec_all = m.sbuf("l_dec", [128, 16, 2, 4], F32, es)
            fl_all = m.sbuf("l_fl", [128, 16, 2, 4], F32, es)
            xc_s = [T(self.ml_xc_d.h, "xc_s%d" % i) for i in range(16)]
            xm_s = [T(self.ml_xm_d.h, "xm_s%d" % i) for i in range(16)]
            zs_s = [T(self.ml_zs_d.h, "zs_s%d" % i) for i in range(16)]
            with ExitStack() as es1:
                hT = m.sbuf("l_hT", [128, KC, S], BF16, es1)
                with ExitStack() as es2:
                    self.norm_mod(xT, hT, l, 0, b, PS4, PS5, es2)
                    m.barrier()
                bd = m.sbuf("l_bd", [128, 3, 16, 128], BF16, es1)
                gw = m.sbuf("l_gw", [128, 3, 16, 16], BF16, es1)
                for src in range(3):
                    m.dma("pool", bd[:, src, :, :], self.ml_bd_d[:, src, :, :])
                    m.dma("pool", gw[:, src, :, :], self.ml_gw_d[:, src, :, :])
                pre_ps = PS7.v(PS7.h[:, 0:256].rearrange("p (t c) -> p t c", t=16))
                m.memset("dve", PS7[:, 0:256], 0.0)
                wb = [m.sbuf("l_w%d" % i, [128, KC, 2, 128], BF16, es1) for i in range(2)]
                xm = [m.sbuf("l_xm%d" % i, [128, S + 4], F32, es1) for i in range(2)]
                cv = [m.sbuf("l_cv%d" % i, [128, S], F32, es1) for i in range(2)]
                zs = [m.sbuf("l_zs%d" % i, [128, S], BF16, es1) for i in range(2)]
                xc = [m.sbuf("l_xc%d" % i, [128, S], BF16, es1) for i in range(2)]
                xmb = [m.sbuf("l_xmb%d" % i, [128, S], BF16, es1) for i in range(2)]
                qkv = [m.sbuf("l_qkv%d" % i, [128, 3, S], BF16, es1) for i in range(2)]
                for i in range(2):
                    m.memset("dve", xm[i][:, 0:2], 0.0)
                    m.memset("dve", xm[i][:, S + 2:S + 4], 0.0)
                winv = self.ml_w_in_d.h.rearrange("(kc p) n -> p kc n", p=128)

                def load_w(fc):
                    m.dma("pool", wb[fc % 2][:, :, 0, :], self.ml_w_in_d.v(winv[:, :, fc * 128:(fc + 1) * 128]))
                    m.dma("pool", wb[fc % 2][:, :, 1, :],
                          self.ml_w_in_d.v(winv[:, :, 2048 + fc * 128:2048 + (fc + 1) * 128]))

                load_w(0)
                load_w(1)
                pctr = [0]

                def inproj(fc):
                    w = wb[fc % 2]
                    xm_, zs_ = xm[fc % 2], zs[fc % 2]
                    for blk in range(NBLK):
                        sl = slice(blk * 512, (blk + 1) * 512)
                        p1 = bank(pctr[0] % 4)
                        pctr[0] += 1
                        for kc in range(KC):
                            m.mm(p1, w[:, kc, 0, :], hT[:, kc, sl], start=(kc == 0), stop=(kc == KC - 1))
                        m.copy("act", xm_[:, 2 + blk * 512:2 + (blk + 1) * 512], p1)
                        p2 = bank(pctr[0] % 4)
                        pctr[0] += 1
                        for kc in range(KC):
                            m.mm(p2, w[:, kc, 1, :], hT[:, kc, sl], start=(kc == 0), stop=(kc == KC - 1))
                        m.act(zs_[:, sl], p2, AF.Silu)
                    if fc + 2 < 16:
                        load_w(fc + 2)

                inproj(0)
                pi = 0
                for fc in range(16):
                    if fc + 1 < 16:
                        inproj(fc + 1)
                    xm_, cv_, zs_, xc_, xmb_, qkv_ = (xm[fc % 2], cv[fc % 2], zs[fc % 2], xc[fc % 2], xmb[fc % 2],
                                                      qkv[fc % 2])
                    for blk in range(0):
                        sl = slice(blk * 512, (blk + 1) * 512)
                        p1 = bank(pi % 4)
                        pi += 1
                        for kc in range(KC):
                            m.mm(p1, w[:, kc, 0, :], hT[:, kc, sl], start=(kc == 0), stop=(kc == KC - 1))
                        m.copy("act", xm_[:, 2 + blk * 512:2 + (blk + 1) * 512], p1)
                        p2 = bank(pi % 4)
                        pi += 1
                        for kc in range(KC):
                            m.mm(p2, w[:, kc, 1, :], hT[:, kc, sl], start=(kc == 0), stop=(kc == KC - 1))
                        m.act(zs_[:, sl], p2, AF.Silu)
                    m.ts("dve", cv_[:], xm_[:, 0:S], lvec[:, fc, 0:1], None, ALU.mult)
                    for j in range(1, 5):
                        m.stt("dve", cv_[:], xm_[:, j:j + S], lvec[:, fc, j:j + 1], cv_[:], ALU.mult, ALU.add)
                    m.act(xc_[:], cv_[:], AF.Silu, bias=lvec[:, fc, 5:6])
                    m.copy("pool", xmb_[:], xm_[:, 2:2 + S])
                    m.dma("sp", zs_s[fc].v(self.ml_zs_d.h[fc]), zs_[:])
                    m.dma("sp", xc_s[fc].v(self.ml_xc_d.h[fc]), xc_[:])
                    m.dma("sp", xm_s[fc].v(self.ml_xm_d.h[fc]), xmb_[:])
                    for blk in range(NBLK):
                        sl = slice(blk * 512, (blk + 1) * 512)
                        for src in range(3):
                            p1 = bank(pctr[0] % 4)
                            pctr[0] += 1
                            rhs = xmb_[:, sl] if src == 2 else xc_[:, sl]
                            m.mm(p1, bd[:, src, fc, :], rhs)
                            if src == 1:
                                m.copy("dve", qkv_[:, src, sl], p1)
                            else:
                                m.copy("act", qkv_[:, src, sl], p1)
                    for t in range(NT):
                        for src in range(3):
                            m.emit("pe", lambda t=t, src=src, qkv_=qkv_: nc.tensor.matmul(
                                pre_ps.ap[:, t, :], qkv_.h[:, src, t * 128:(t + 1) * 128], gw.h[:, src, fc, :],
                                start=False, stop=False, skip_group_check=True),
                                [qkv_[:], gw[:]], [PS7[:]])
                gb_bc = V(bass.AP(gb.h, 0, [[gb.h[:].ap[0][0], 128], [0, 16], [1, 16]]), (gb.buf,))
                m.tt("dve", pre.v(pre.h[:].rearrange("p t d g -> p t (d g)")), pre_ps, gb_bc, ALU.add)
                self.dump("l_pre", pre[:], [128, 16, 2, 8], F32)
                m.barrier()
            with ExitStack() as es1:
                def sbt(name, shape=(128, 16, 2, 4)):
                    return m.sbuf(name, list(shape), F32, es1)
                ex, lp, u, nb, ubc, a_all, darg, garg, farg = (sbt("g_ex"), sbt("g_lp"), sbt("g_u"), sbt("g_nb"),
                                                                sbt("g_ubc"), sbt("g_a"), sbt("g_darg"),
                                                                sbt("g_garg"), sbt("g_farg"))
                nbL = sbt("g_nbL")
                umax = sbt("g_umax", (128, 1))
                dg = sbt("g_dg", (128, 128))
                m_all = sbt("g_m", (128, 17, 2, 4))
                fcols = pre[:, :, :, 4:8]
                icols = pre[:, :, :, 0:4]
                m.act(ex[:], fcols, AF.Exp, scale=-1.0)
                m.act(lp[:], ex[:], AF.Ln, bias=self.ones_f[:, 0:1])
                lp2 = lp.v(lp.h[:].rearrange("p t d h -> p (t d h)"))
                pf, pb, pl, pt_, pu = bank(0), bank(1), bank(2), bank(3), PS4[:, 0:128]
                m.mm(banks[0].v(dCps.h[:, 0, 0:128]), self.cst[:, 3, :], lp2)
                m.mm(banks[1].v(dCps.h[:, 1, 0:128]), self.cst[:, 4, :], lp2)
                m.mm(banks[2].v(dCps.h[:, 2, 0:128]), self.ones_f, lp2)

                def v4(bk, j):
                    return bk.v(dCps.h[:, j, 0:128].rearrange("p (t d h) -> p t d h", t=16, d=2))
                m.copy("dve", nb[:, :, 0, :], V(v4(banks[0], 0).ap[:, :, 0, :], (banks[0].buf,)))
                m.copy("dve", nb[:, :, 1, :], V(v4(banks[1], 1).ap[:, :, 1, :], (banks[1].buf,)))
                m.copy("dve", nbL[:], v4(banks[2], 2))
                m.tt("dve", u[:], icols, nb[:], ALU.add)
                u2 = u.v(u.h[:].rearrange("p t d h -> p (t d h)"))
                m.transpose(banks[3].v(dCps.h[:, 3, 0:128]), u2, self.ident)
                m.reduce("dve", umax[:], banks[3].v(dCps.h[:, 3, 0:128]), ALU.max)
                m.ts("dve", dg[:], self.ident, umax[:], None, ALU.mult)
                m.mm(pu, self.ones_f, dg[:])
                m.copy("dve", ubc.v(ubc.h[:].rearrange("p t d h -> p (t d h)")), pu)
                m.memset("dve", m_all[:, 0, 0, :], 0.0)
                m.memset("dve", m_all[:, 16, 1, :], 0.0)
                for st in range(16):
                    for d_ in range(2):
                        c = st if d_ == 0 else 15 - st
                        src_i = c if d_ == 0 else c + 1
                        dst_i = c + 1 if d_ == 0 else c
                        m.tt("dve", a_all[:, c, d_, :], m_all[:, src_i, d_, :], ubc[:, c, d_, :], ALU.max)
                        m.tt("dve", darg[:, c, d_, :], m_all[:, src_i, d_, :], a_all[:, c, d_, :], ALU.subtract)
                        m.tt("dve", m_all[:, dst_i, d_, :], a_all[:, c, d_, :], nbL[:, c, d_, :], ALU.subtract)
                m.tt("dve", garg[:], u[:], a_all[:], ALU.subtract)
                m.tt("dve", farg[:], nb[:], a_all[:], ALU.subtract)
                m.act(g_all[:], garg[:], AF.Exp)
                m.act(dec_all[:], darg[:], AF.Exp)
                m.act(fl_all[:], farg[:], AF.Exp)
                self.dump("l_g", g_all[:], [128, 16, 2, 4], F32)
                self.dump("l_dec", dec_all[:], [128, 16, 2, 4], F32)
                self.dump("l_fl", fl_all[:], [128, 16, 2, 4], F32)
                m.barrier()
            wov = self.ml_w_out_d.h.rearrange("(c p) n -> p c n", p=128)
            bdh = m.sbuf("l_bdh", [128, 3, 4, 128], BF16, es)
            wo = [m.sbuf("l_wo%d" % i, [128, 4, D], BF16, es) for i in range(1)]
            qT = m.sbuf("l_qT", [128, 4, S], BF16, es)
            kT = m.sbuf("l_kT", [128, 4, S], BF16, es)
            ktm = m.sbuf("l_ktm", [128, NT, 512], BF16, es)
            vtm = m.sbuf("l_vtm", [128, NT, 512], BF16, es)
            hfw = m.sbuf("l_hfw", [128, NT, 512], BF16, es)
            Cst = m.sbuf("l_C", [128, 4, 512], F32, es)
            Cbs = [m.sbuf("l_Cb%d" % i, [128, 4, 512], BF16, es) for i in range(3)]
            nst = m.sbuf("l_n", [128, 4], F32, es)
            nbfs = [m.sbuf("l_nb%d" % i, [128, 4], BF16, es) for i in range(3)]
            xin = [m.sbuf("l_xin%d" % i, [128, 4, 512], BF16, es) for i in range(1)]
            xmin = [m.sbuf("l_xmin%d" % i, [128, 4, 512], BF16, es) for i in range(1)]
            xce = [m.sbuf("l_xce%d" % i, [128, 4, 128], BF16, es) for i in range(2)]
            zse = [m.sbuf("l_zse%d" % i, [128, 4, 128], BF16, es) for i in range(2)]
            PT = [m.sbuf("l_PT%d" % i, [128, 128], BF16, es) for i in range(2)]
            qp = [m.sbuf("l_qp%d" % i, [128, 4, 128], BF16, es) for i in range(2)]
            kw = [m.sbuf("l_kw%d" % i, [128, 512], BF16, es) for i in range(2)]
            rd = [m.sbuf("l_rd%d" % i, [128, 1], F32, es) for i in range(2)]
            ss = [m.sbuf("l_ss%d" % i, [128, 1], F32, es) for i in range(2)]
            t1 = [m.sbuf("l_t1%d" % i, [128, 128], F32, es) for i in range(2)]
            t2 = [m.sbuf("l_t2%d" % i, [128, 4, 128], F32, es) for i in range(1)]
            yT = [m.sbuf("l_yT%d" % i, [128, 4, 512], BF16, es) for i in range(1)]
            sT = PS7[:, 0:128]
            den = PS5[:, 0:1]
            dn = PS5[:, 8:12]
            ones_col = self.ones_bf
            xib = [xin[0], yT[0]]
            xmb2 = [xmin[0], Cbs[0]]

            def load_bd(h):
                for src in range(3):
                    m.dma("pool", bdh[:, src, :, :], self.ml_bd_d[:, src, 4 * h:4 * h + 4, :])

            load_bd(0)
            for h in range(4):
                m.dma("pool", wo[0][:], self.ml_w_out_d.v(wov[:, 4 * h:4 * h + 4, :]))

                def load_blk(blk):
                    sl_ = slice(blk * 512, (blk + 1) * 512)
                    for j in range(4):
                        fc = 4 * h + j
                        m.dma("sp", xib[blk % 2][:, j, :], xc_s[fc].v(self.ml_xc_d.h[fc][:, sl_]))
                        m.dma("sp", xmb2[blk % 2][:, j, :], xm_s[fc].v(self.ml_xm_d.h[fc][:, sl_]))

                pj = 0
                load_blk(0)
                for blk in range(NBLK):
                    sl = slice(blk * 512, (blk + 1) * 512)
                    if blk + 1 < NBLK:
                        load_blk(blk + 1)
                    xi, xmi = xib[blk % 2], xmb2[blk % 2]
                    for j in range(4):
                        fc = 4 * h + j
                        p1 = (PS4, PS5)[pj % 2]
                        pj += 1
                        m.mm(p1[:], bdh[:, 0, j, :], xi[:, j, :])
                        m.copy("dve" if j % 2 else "act", qT[:, j, sl], p1[:])
                        p1 = (PS4, PS5)[pj % 2]
                        pj += 1
                        m.mm(p1[:], bdh[:, 1, j, :], xi[:, j, :])
                        m.act(kT[:, j, sl], p1[:], AF.Copy, scale=ISQ)
                    for tt4 in range(4):
                        t = blk * 4 + tt4
                        tl = slice(tt4 * 128, (tt4 + 1) * 128)
                        p1 = (PS4, PS5)[pj % 2]
                        pj += 1
                        for j in range(4):
                            m.mm(p1[:, j * 128:(j + 1) * 128], xi[:, j, tl], bdh[:, 1, j, :])
                        m.ts("dve", ktm[:, t, :], p1[:], ISQ, None, ALU.mult)
                        p1 = (PS4, PS5)[pj % 2]
                        pj += 1
                        for j in range(4):
                            m.mm(p1[:, j * 128:(j + 1) * 128], xmi[:, j, tl], bdh[:, 2, j, :])
                        m.copy("dve", vtm[:, t, :], p1[:])
                if h + 1 < 4:
                    load_bd(h + 1)
                for d_ in range(2):
                    m.memset("dve", Cst[:], 0.0)
                    m.memset("pool", Cbs[2][:], 0.0)
                    m.memset("dve", nst[:], 0.0)
                    m.memset("pool", nbfs[2][:], 0.0)
                    mask = self.cst[:, 3 + d_, :]

                    def chunk(st):
                        return st if d_ == 0 else 15 - st

                    def partA(st):
                        c = chunk(st)
                        tsl = slice(c * 128, (c + 1) * 128)
                        gcol = g_all[:, c, d_, h:h + 1]
                        dcol = dec_all[:, c, d_, h:h + 1]
                        for j in range(4):
                            m.mm(sT, kT[:, j, tsl], qT[:, j, tsl], start=(j == 0), stop=(j == 3))
                        P_ = PT[st % 2]
                        m.stt("dve", P_[:], sT, gcol, mask, ALU.mult, ALU.mult)
                        k_ = kw[st % 2]
                        m.ts("dve", k_[:], ktm[:, c, :], gcol, None, ALU.mult)
                        q_ = qp[st % 2]
                        m.act(q_[:], qT[:, :, tsl], AF.Copy, scale=dcol)
                        for j in range(4):
                            m.mm(V(dn.ap[:, j:j + 1], dn.bufs), k_[:, j * 128:(j + 1) * 128], ones_col[:, 0:1])
                        for j in range(4):
                            m.mm(dCps[:, j, :], k_[:, j * 128:(j + 1) * 128], vtm[:, c, :])
                        m.stt("dve", nst[:], nst[:], dcol, dn, ALU.mult, ALU.add)
                        m.copy("dve", nbfs[st % 3][:], nst[:])
                        m.stt("dve", Cst[:], Cst[:], dcol, dCps[:], ALU.mult, ALU.add)

                    def partB(st):
                        c = chunk(st)
                        fcol = fl_all[:, c, d_, h:h + 1]
                        Cb = Cbs[(st + 2) % 3]
                        nbf = nbfs[(st + 2) % 3]
                        P_ = PT[st % 2]
                        q_ = qp[st % 2]
                        m.mm(PS4[:], P_[:], vtm[:, c, :], start=True, stop=False)
                        for j in range(4):
                            m.mm(PS4[:], q_[:, j, :], Cb[:, j, :], start=False, stop=(j == 3))
                        m.mm(den, P_[:], ones_col[:, 0:1], start=True, stop=False)
                        for j in range(4):
                            m.mm(den, q_[:, j, :], nbf[:, j:j + 1], start=False, stop=(j == 3))
                        r_ = rd[st % 2]
                        m.act(r_[:], den, AF.Abs)
                        m.ts("dve", r_[:], r_[:], fcol, None, ALU.max)
                        m.recip(r_[:], r_[:])
                        if d_ == 0:
                            m.act(hfw[:, c, :], PS4[:], AF.Copy, scale=r_[:])
                        else:
                            m.stt("dve", hfw[:, c, :], PS4[:], r_[:], hfw[:, c, :], ALU.mult, ALU.add)

                    partA(0)
                    m.copy("act", Cbs[0][:], Cst[:])
                    for st in range(16):
                        if st + 1 < 16:
                            partA(st + 1)
                        partB(st)
                        if st + 1 < 16:
                            m.copy("act", Cbs[(st + 1) % 3][:], Cst[:])
                hnb = [xmin[0][:, 1, :], xmin[0][:, 0, :]]
                yTb = [yT[0], xin[0]]

                def E1(c):
                    tsl = slice(c * 128, (c + 1) * 128)
                    for j in range(4):
                        fc = 4 * h + j
                        m.dma("sp", xce[c % 2][:, j, :], xc_s[fc].v(self.ml_xc_d.h[fc][:, tsl]))
                        m.dma("sp", zse[c % 2][:, j, :], zs_s[fc].v(self.ml_zs_d.h[fc][:, tsl]))
                    s_ = ss[c % 2]
                    hn_ = hnb[c % 2]
                    m.act(hn_, hfw[:, c, :], AF.Square, accum_out=s_[:])
                    m.act(s_[:], s_[:], AF.Ln, bias=self.epsT[:], scale=1.0 / 512)
                    m.act(s_[:], s_[:], AF.Exp, scale=-0.5)
                    m.act(hn_, hfw[:, c, :], AF.Copy, scale=s_[:])

                def E2(c):
                    hn_ = hnb[c % 2]
                    for j in range(4):
                        m.transpose(PS6[:, j, :], hn_[:, j * 128:(j + 1) * 128], self.ident_bf)
                    t1_, t2_ = t1[0], t2[0]
                    y_ = yTb[(c // 4) % 2]
                    csl = slice((c % 4) * 128, (c % 4 + 1) * 128)
                    for j in range(4):
                        fc = 4 * h + j
                        m.ts("dve", t2_[:, j, :], xce[c % 2][:, j, :], lvec[:, fc, 6:7], None, ALU.mult)
                        m.stt("dve", t2_[:, j, :], PS6[:, j, :], lvec[:, fc, 7:8], t2_[:, j, :],
                              ALU.mult, ALU.add)
                    m.tt("dve", V(y_.h[:, :, csl], (y_.buf,)), t2_[:], zse[c % 2][:], ALU.mult)
                    if c % 4 == 3:
                        blk = c // 4
                        bsl = slice(blk * 512, (blk + 1) * 512)
                        for oc in range(KC):
                            pp = (PS5, PS4)[oc % 2]
                            for j in range(4):
                                m.mm(pp[:], wo[0][:, j, oc * 128:(oc + 1) * 128], y_[:, j, :],
                                     start=(j == 0), stop=(j == 3))
                            m.stt("dve", xT[:, oc, bsl], pp[:], self.mod[:, l, 16 + oc, b:b + 1],
                                  xT[:, oc, bsl], ALU.mult, ALU.add)

                for stp in range(17):
                    if stp < 16:
                        E1(stp)
                    if stp >= 1:
                        E2(stp - 1)
            m.barrier()

    def build(self, stages=("attn", "moe0", "mlstm", "moe1")):
        m = self.m
        self.declare(stages)
        self.consts()
        self.prologue()
        for b in range(NB):
            with ExitStack() as es:
                xT = m.sbuf("xT", [128, KC, S], F32, es)
                with ExitStack() as es2:
                    PS = [m.psum("iops%d" % i, [128, 512], F32, es2) for i in range(4)]
                    self.load_x(b, xT, PS)
                if "attn" in stages:
                    self.attention(xT, b)
                if "moe0" in stages:
                    self.moe(xT, b, 0)
                if "mlstm" in stages:
                    self.mlstm(xT, b)
                if "moe1" in stages:
                    self.moe(xT, b, 1)
                with ExitStack() as es2:
                    PS = [m.psum("iops%d" % i, [128, 512], F32, es2) for i in range(4)]
                    self.store_x(b, xT, PS)
        m.finish()
        self.es.close()
        return self.nc


def _fm(v, nchunk):
    return np.ascontiguousarray(np.asarray(v, np.float32).reshape(nchunk, 128).T)


def host_layout(inp, core):
    f32 = np.float32
    d = {}
    bs = slice(core * NB, (core + 1) * NB)
    d["x"] = np.ascontiguousarray(inp["x"][bs])
    c = np.asarray(inp["c"][bs], f32)
    d["cT"] = np.ascontiguousarray(c.T.reshape(KC, 128, NB).transpose(1, 0, 2))
    d["ada_w"] = inp["ada_w"]
    d["ada_bT"] = np.ascontiguousarray(np.asarray(inp["ada_b"], f32).reshape(2, 48, 128).transpose(2, 0, 1))
    ng = np.stack([inp["norm_mix_g"], inp["norm_ffn_g"]], axis=1)
    d["normgT"] = np.ascontiguousarray(np.asarray(ng, f32).reshape(2, 2, KC, 128).transpose(3, 0, 1, 2))
    cst = np.zeros((128, 6, 128), f32)
    cst[:, 0, :] = np.eye(128)
    cst[:, 1, :] = 1.0
    cst[0:64, 2, 0:64] = 1.0
    cst[64:128, 2, 64:128] = 1.0
    ii = np.arange(128)
    cst[:, 3, :] = (ii[:, None] <= ii[None, :])
    cst[:, 4, :] = (ii[:, None] >= ii[None, :])
    d["consts"] = cst
    d["da_w_in"] = inp["da_w_in"][0]
    d["da_w_out"] = inp["da_w_out"][0]
    dv = np.zeros((128, 4), f32)
    dv[:, 0] = np.tile(inp["da_q_gain"][0], 2)
    dv[:, 1] = np.tile(inp["da_k_gain"][0], 2)
    dv[:, 2] = inp["da_subln_g"][0]
    d["da_vec"] = dv
    lam = np.stack([inp["da_lam_q1"][0], inp["da_lam_k1"][0], inp["da_lam_q2"][0], inp["da_lam_k2"][0]])
    d["da_lam"] = np.ascontiguousarray(np.broadcast_to(lam[None], (128, 4, 64)).astype(f32))
    i_ = np.arange(128)[:, None]
    c_ = np.arange(1152)[None, :]
    idx = _t5_bucket(i_ - c_ + 512)
    tab = np.asarray(inp["rel_table"], f32)
    d["da_G"] = np.ascontiguousarray(tab[idx].transpose(2, 0, 1))
    d["da_cb"] = np.ascontiguousarray(np.broadcast_to(tab[[15, 31]].T[None], (128, 8, 2)).astype(f32))
    d["ml_w_in"] = inp["ml_w_in"][0]
    d["ml_w_out"] = inp["ml_w_out"][0]
    mv = np.zeros((128, 16, 8), f32)
    for j in range(5):
        mv[:, :, j] = _fm(inp["ml_conv_w"][0][j], 16)
    mv[:, :, 5] = _fm(inp["ml_conv_b"][0], 16)
    mv[:, :, 6] = _fm(inp["ml_skip"][0], 16)
    mv[:, :, 7] = _fm(np.asarray(inp["ml_outnorm_g"][0]).reshape(-1), 16)
    d["ml_vec"] = mv
    bdm = np.zeros((128, 3, 16, 128), f32)
    pin = np.arange(128)
    for si, nm in enumerate(("ml_wq", "ml_wk", "ml_wv")):
        w = np.asarray(inp[nm][0], f32)
        for c in range(16):
            blk = w[c * 32:(c + 1) * 32]
            for o in range(4):
                bdm[pin, si, c, (pin // 4) * 4 + o] = blk[pin // 4, pin % 4, o]
    d["ml_bd"] = bdm
    gwt = np.asarray(inp["ml_gate_w"][0], f32)
    d["ml_gw"] = np.ascontiguousarray(gwt.reshape(2, 3, 16, 128, 8).transpose(3, 1, 2, 0, 4).reshape(128, 3, 16, 16))
    d["ml_gb"] = np.ascontiguousarray(np.broadcast_to(np.asarray(inp["ml_gate_b"][0], f32).reshape(1, 16), (128, 16)))
    d["moe_wr"] = np.ascontiguousarray(np.concatenate([inp["moe_w_group"], inp["moe_w_router"]], axis=2))
    d["moe_w1"] = inp["moe_w1"]
    d["moe_w3"] = inp["moe_w3"]
    d["moe_w2"] = inp["moe_w2"]
    return d


_NC_CACHE = {}


def kernel(**inputs):
    inp = {k: np.asarray(v) for k, v in inputs.items()}
    if "nc" not in _NC_CACHE:
        _NC_CACHE["nc"] = K().build()
    nc = _NC_CACHE["nc"]
    shared = None
    in_maps = []
    for core in range(8):
        d = host_layout(inp, core)
        if shared is None:
            shared = d
        else:
            for k in d:
                if k not in ("x", "cT"):
                    d[k] = shared[k]
        in_maps.append({k: np.ascontiguousarray(v, dtype=np.float32) for k, v in d.items()})
    res = run_bass_kernel_spmd(nc, in_maps, core_ids=list(range(8)))
    out = np.concatenate([r["out"] for r in res.results], axis=0)
    return out.astype(np.float32)
```

```python
import numpy as np
from contextlib import ExitStack
import concourse.bass as bass
import concourse.mybir as mybir

F32 = mybir.dt.float32
BF16 = mybir.dt.bfloat16
ALU = mybir.AluOpType
AF = mybir.ActivationFunctionType
AX = mybir.AxisListType


class Buf:
    __slots__ = ("name", "w", "r")

    def __init__(self, name):
        self.name = name
        self.w = None
        self.r = {}


class V:
    __slots__ = ("ap", "bufs")

    def __init__(self, ap, bufs):
        self.ap = ap
        self.bufs = bufs

    def __getitem__(self, idx):
        return V(self.ap[idx], self.bufs)


class T:
    def __init__(self, handle, name, buf=None):
        self.h = handle
        self.buf = buf if buf is not None else Buf(name)

    def __getitem__(self, idx):
        return V(self.h[idx], (self.buf,))

    def v(self, ap):
        return V(ap, (self.buf,))


class Mach:
    NRING = {"sp": 12, "act": 4, "pool": 8}

    def __init__(self, nc, es):
        self.nc = nc
        self.es = es
        self.engs = {"pe": nc.tensor, "act": nc.scalar, "dve": nc.vector,
                     "pool": nc.gpsimd, "sp": nc.sync}
        self.sem = {}
        self.cnt = {}
        for e in ("pe", "act", "dve", "pool"):
            self.sem[e] = es.enter_context(nc.semaphore("c_" + e))
            self.cnt[e] = 0
        self.ring = {}
        self.ring_pos = {}
        for q, n in self.NRING.items():
            self.ring[q] = []
            for i in range(n):
                k = ("dma", q, i)
                self.sem[k] = es.enter_context(nc.semaphore("d_%s%d" % (q, i)))
                self.cnt[k] = 0
                self.ring[q].append(k)
            self.ring_pos[q] = 0
        self.waited = {e: {} for e in self.engs}
        self.ninst = 0

    def _uniq(self, name):
        self._uid = getattr(self, "_uid", 0) + 1
        return "%s_u%d" % (name, self._uid)

    def sbuf(self, name, shape, dtype, es=None):
        name = self._uniq(name)
        h = (es or self.es).enter_context(self.nc.sbuf_tensor(name, list(shape), dtype))
        return T(h, name)

    def psum(self, name, shape, dtype, es=None):
        name = self._uniq(name)
        h = (es or self.es).enter_context(self.nc.psum_tensor(name, list(shape), dtype))
        return T(h, name)

    def _wait(self, eng, tok):
        key, val = tok
        if self.waited[eng].get(key, 0) >= val:
            return
        self.engs[eng].wait_ge(self.sem[key], val)
        self.waited[eng][key] = val

    def _deps(self, eng, reads, writes):
        toks = []
        for v in reads:
            for b in v.bufs:
                if b.w is not None:
                    toks.append(b.w)
        for v in writes:
            for b in v.bufs:
                if b.w is not None and b.w[0] != eng:
                    toks.append(b.w)
                for k, t in b.r.items():
                    if k != eng:
                        toks.append(t)
        for t in toks:
            self._wait(eng, t)

    def _commit(self, tok, reads, writes):
        for v in reads:
            for b in v.bufs:
                b.r[tok[0]] = tok
        for v in writes:
            for b in v.bufs:
                b.w = tok
                b.r = {}

    def emit(self, eng, fn, reads, writes):
        self._deps(eng, reads, writes)
        inst = fn()
        self.cnt[eng] += 1
        inst.then_inc(self.sem[eng], 1)
        tok = (eng, self.cnt[eng])
        self._commit(tok, reads, writes)
        self.ninst += 1
        return tok

    def dma(self, q, out, in_, **kw):
        ring = self.ring[q]
        key = ring[self.ring_pos[q] % len(ring)]
        self.ring_pos[q] += 1
        if self.cnt[key] > 0:
            self._wait(q, (key, self.cnt[key]))
        self._deps(q, [in_], [out])
        inst = self.engs[q].dma_start(out=out.ap, in_=in_.ap, **kw)
        self.cnt[key] += 16
        inst.then_inc(self.sem[key], 16)
        tok = (key, self.cnt[key])
        self._commit(tok, [in_], [out])
        self.ninst += 1
        return tok

    def barrier(self):
        for e in self.engs:
            for k, c in self.cnt.items():
                if c > 0 and k != e:
                    self._wait(e, (k, c))

    def finish(self):
        for k, c in self.cnt.items():
            if c > 0:
                self._wait("sp", (k, c))

    def mm(self, out, lhsT, rhs, start=True, stop=True, extra_w=()):
        return self.emit("pe", lambda: self.nc.tensor.matmul(out.ap, lhsT.ap, rhs.ap, start=start, stop=stop),
                         [lhsT, rhs], [out])

    def transpose(self, out, in_, ident):
        return self.emit("pe", lambda: self.nc.tensor.transpose(out.ap, in_.ap, ident.ap),
                         [in_, ident], [out])

    def act(self, out, in_, func, bias=None, scale=None, accum_out=None, eng="act"):
        reads = [in_]
        kw = {}
        if bias is not None:
            if isinstance(bias, V):
                reads.append(bias)
                kw["bias"] = bias.ap
            else:
                kw["bias"] = bias
        if scale is not None:
            if isinstance(scale, V):
                reads.append(scale)
                kw["scale"] = scale.ap
            else:
                kw["scale"] = scale
        writes = [out]
        if accum_out is not None:
            writes.append(accum_out)
            kw["accum_out"] = accum_out.ap
        return self.emit("act", lambda: self.nc.scalar.activation(out.ap, in_.ap, func, **kw), reads, writes)

    def tt(self, eng, out, in0, in1, op):
        e = self.engs[eng]
        return self.emit(eng, lambda: e.tensor_tensor(out.ap, in0.ap, in1.ap, op), [in0, in1], [out])

    def ts(self, eng, out, in0, s1, s2, op0, op1=None, accum_out=None):
        e = self.engs[eng]
        reads = [in0]
        a1 = s1
        a2 = s2
        if isinstance(s1, V):
            reads.append(s1)
            a1 = s1.ap
        if isinstance(s2, V):
            reads.append(s2)
            a2 = s2.ap
        writes = [out]
        kw = {}
        if op1 is not None:
            kw["op1"] = op1
        if accum_out is not None:
            writes.append(accum_out)
            kw["accum_out"] = accum_out.ap
        return self.emit(eng, lambda: e.tensor_scalar(out.ap, in0.ap, a1, a2, op0, **kw), reads, writes)

    def stt(self, eng, out, in0, scalar, in1, op0, op1):
        e = self.engs[eng]
        reads = [in0, in1]
        a = scalar
        if isinstance(scalar, V):
            reads.append(scalar)
            a = scalar.ap
        return self.emit(eng, lambda: e.scalar_tensor_tensor(out.ap, in0.ap, a, in1.ap, op0, op1), reads, [out])

    def copy(self, eng, out, in_):
        if eng == "act":
            return self.emit("act", lambda: self.nc.scalar.copy(out.ap, in_.ap), [in_], [out])
        e = self.engs[eng]
        return self.emit(eng, lambda: e.tensor_copy(out.ap, in_.ap), [in_], [out])

    def reduce(self, eng, out, in_, op, axis=AX.X):
        e = self.engs[eng]
        return self.emit(eng, lambda: e.tensor_reduce(out.ap, in_.ap, axis, op), [in_], [out])

    def memset(self, eng, out, val):
        e = self.engs[eng]
        return self.emit(eng, lambda: e.memset(out.ap, val), [], [out])

    def recip(self, out, in_):
        return self.emit("dve", lambda: self.nc.vector.reciprocal(out.ap, in_.ap), [in_], [out])


import math
from concourse.bass_utils import run_bass_kernel_spmd
import ml_dtypes

NB = 2
S = 2048
D = 1024
KC = 8
NT = 16
NBLK = 4
EPS = 1e-6
LAM_INIT0 = 0.8 - 0.6 * math.exp(-0.3 * 0)


def _t5_bucket(rel):
    nb = 16
    me = 8
    ret = np.where(rel > 0, nb, 0)
    n = np.abs(rel)
    nf = np.maximum(n, 1).astype(np.float32)
    large = me + (np.log(nf / np.float32(me)) / np.float32(math.log(128 / me)) * np.float32(nb - me)).astype(np.int32)
    large = np.minimum(large, nb - 1)
    return ret + np.where(n < me, n, large)


class K:
    def __init__(self, dbg=None):
        self.dbg = dbg
        nc = bass.Bass("TRN2", target_bir_lowering=False)
        self.nc = nc
        self.es = ExitStack()
        self.m = Mach(nc, self.es)
        self.din = {}

    def dram_in(self, name, shape, dtype=F32):
        h = self.nc.dram_tensor(name, list(shape), dtype, kind="ExternalInput")
        t = T(h.ap(), name)
        self.din[name] = t
        return t

    def dram_out(self, name, shape, dtype=F32):
        h = self.nc.dram_tensor(name, list(shape), dtype, kind="ExternalOutput")
        return T(h.ap(), name)

    def dram_tmp(self, name, shape, dtype=F32):
        h = self.nc.dram_tensor(name, list(shape), dtype, kind="Internal")
        return T(h.ap(), name)

    def declare(self, stages):
        di = self.dram_in
        self.x_d = di("x", [NB, S, D])
        self.cT_d = di("cT", [128, KC, NB])
        self.ada_w_d = di("ada_w", [2, D, 6 * D])
        self.ada_b_d = di("ada_bT", [128, 2, 48])
        self.normg_d = di("normgT", [128, 2, 2, KC])
        self.consts_d = di("consts", [128, 6, 128])
        self.da_w_in_d = di("da_w_in", [D, 3 * D])
        self.da_w_out_d = di("da_w_out", [D, D])
        self.da_vec_d = di("da_vec", [128, 4])
        self.da_lam_d = di("da_lam", [128, 4, 64])
        self.da_G_d = di("da_G", [8, 128, 1152])
        self.da_cb_d = di("da_cb", [128, 8, 2])
        if "moe0" in stages or "moe1" in stages:
            self.moe_wr_d = di("moe_wr", [2, D, 36])
            self.moe_w1_d = di("moe_w1", [2, 32, D, 256])
            self.moe_w3_d = di("moe_w3", [2, 32, D, 256])
            self.moe_w2_d = di("moe_w2", [2, 32, 256, D])
        if "mlstm" in stages:
            self.ml_w_in_d = di("ml_w_in", [D, 4 * D])
            self.ml_w_out_d = di("ml_w_out", [2 * D, D])
            self.ml_vec_d = di("ml_vec", [128, 16, 8])
            self.ml_bd_d = di("ml_bd", [128, 3, 16, 128])
            self.ml_gw_d = di("ml_gw", [128, 3, 16, 16])
            self.ml_gb_d = di("ml_gb", [128, 16])
            self.ml_xc_d = self.dram_tmp("ml_xc_scr", [16, 128, S], BF16)
            self.ml_xm_d = self.dram_tmp("ml_xm_scr", [16, 128, S], BF16)
            self.ml_zs_d = self.dram_tmp("ml_zs_scr", [16, 128, S], BF16)
        self.out_d = self.dram_out("out", [NB, S, D])
        self.gT_d = self.dram_tmp("gT_scr", [32, S])

    def dump(self, tag, view, shape, dtype):
        if not self.dbg or tag not in self.dbg:
            return
        if not hasattr(self, "_dumped"):
            self._dumped = set()
        if tag in self._dumped:
            return
        self._dumped.add(tag)
        h = self.nc.dram_tensor("dbg_" + tag, list(shape), dtype, kind="ExternalOutput")
        t = T(h.ap(), "dbg_" + tag)
        self.m.dma("sp", t[:], view)

    def consts(self):
        m = self.m
        self.cst = m.sbuf("cst", [128, 6, 128], F32)
        m.dma("sp", self.cst[:], self.consts_d[:])
        self.cst_bf = m.sbuf("cst_bf", [128, 6, 128], BF16)
        m.copy("dve", self.cst_bf[:], self.cst[:])
        self.ident = self.cst[:, 0, :]
        self.ones_f = self.cst[:, 1, :]
        self.ident_bf = self.cst_bf[:, 0, :]
        self.ones_bf = self.cst_bf[:, 1, :]
        self.bones_bf = self.cst_bf[:, 2, :]
        self.epsT = m.sbuf("epsT", [128, 1], F32)
        m.memset("dve", self.epsT[:], EPS)

    def prologue(self):
        m = self.m
        nc = self.nc
        mod = m.sbuf("mod", [128, 2, 48, NB], F32)
        self.nscale = m.sbuf("nscale", [128, 2, 2, KC, NB], F32)
        with ExitStack() as es:
            cT = m.sbuf("cT", [128, KC, NB], F32, es)
            cact = m.sbuf("cact", [128, KC, NB], BF16, es)
            adab = m.sbuf("adab", [128, 2, 48], F32, es)
            normg = m.sbuf("normg", [128, 2, 2, KC], F32, es)
            wbuf = [m.sbuf("adaw%d" % i, [128, KC, 768], BF16, es) for i in range(2)]
            ps = m.psum("modps", [128, 2, 48, NB], F32, es)
            m.dma("sp", cT[:], self.cT_d[:])
            m.dma("sp", adab[:], self.ada_b_d[:])
            m.dma("sp", normg[:], self.normg_d[:])
            m.act(cact[:], cT[:], AF.Silu)
            i = 0
            for l in range(2):
                wv = self.ada_w_d.h[l].rearrange("(kc p) n -> p kc n", p=128)
                for g in range(8):
                    wb = wbuf[i % 2]
                    i += 1
                    m.dma("pool", wb[:], self.ada_w_d.v(wv[:, :, g * 768:(g + 1) * 768]))
                    for oc in range(6):
                        c = g * 6 + oc
                        for kc in range(KC):
                            m.mm(ps[:, l, c, :], wb[:, kc, oc * 128:(oc + 1) * 128], cact[:, kc, :],
                                 start=(kc == 0), stop=(kc == KC - 1))
            adab_bc = V(bass.AP(adab.h, 0, [[adab.h[:].ap[0][0], 128], [48, 2], [1, 48], [0, NB]]), (adab.buf,))
            m.tt("dve", mod[:], ps[:], adab_bc, ALU.add)
            self.dump("mod", mod[:], [128, 2, 48, NB], F32)
            self.mod = mod
            for l in range(2):
                for j in range(2):
                    sc = mod[:, l, 8 + 24 * j: 16 + 24 * j, :]
                    gsl = normg.h[:, l, j, :]
                    gbc = V(bass.AP(normg.h, gsl.offset, [list(gsl.ap[0]), [1, KC], [0, NB]]), (normg.buf,))
                    m.stt("dve", self.nscale[:, l, j, :, :], sc, 1.0, gbc, ALU.add, ALU.mult)
            m.barrier()

    def mod_vec(self, l, which, b):
        base = {"sh1": 0, "g1": 16, "sh2": 24, "g2": 40}[which]
        return lambda c: self.mod[:, l, base + c, b:b + 1]

    def load_x(self, b, xT, ps_list):
        m = self.m
        with ExitStack() as es:
            xin = [m.sbuf("xin%d" % i, [128, D], F32, es) for i in range(2)]
            for t in range(NT):
                xi = xin[t % 2]
                m.dma("sp", xi[:], self.x_d[b, t * 128:(t + 1) * 128, :])
                for half in range(2):
                    ps = ps_list[(t * 2 + half) % len(ps_list)]
                    for j in range(4):
                        c = half * 4 + j
                        m.transpose(ps[:, j * 128:(j + 1) * 128], xi[:, c * 128:(c + 1) * 128], self.ident)
                    src = ps.v(ps.h[:, :].rearrange("p (j t) -> p j t", j=4))
                    dst = xT[:, half * 4:(half + 1) * 4, t * 128:(t + 1) * 128]
                    if (t + half) % 2 == 0:
                        m.copy("dve", dst, src)
                    else:
                        m.copy("act", dst, src)
            m.barrier()

    def store_x(self, b, xT, ps_list):
        m = self.m
        with ExitStack() as es:
            xo = [m.sbuf("xo%d" % i, [128, D], F32, es) for i in range(2)]
            for t in range(NT):
                xi = xo[t % 2]
                for half in range(2):
                    ps = ps_list[(t * 2 + half) % len(ps_list)]
                    for j in range(4):
                        c = half * 4 + j
                        m.transpose(ps[:, j * 128:(j + 1) * 128], xT[:, c, t * 128:(t + 1) * 128], self.ident)
                    dst = xi[:, half * 512:(half + 1) * 512]
                    if (t + half) % 2 == 0:
                        m.copy("dve", dst, ps[:, :])
                    else:
                        m.copy("act", dst, ps[:, :])
                m.dma("sp", self.out_d[b, t * 128:(t + 1) * 128, :], xi[:])
            m.barrier()

    def norm_mod(self, xT, hT, l, j, b, psA, psB, es):
        m = self.m
        sq = [m.sbuf("nm_sq%d" % i, [128, KC, 512], BF16, es) for i in range(2)]
        rs = [m.sbuf("nm_rs%d" % i, [128, 512], F32, es) for i in range(2)]
        tmp = [m.sbuf("nm_t%d" % i, [128, 512], F32, es) for i in range(3)]
        shbase = 0 if j == 0 else 24
        ti = 0
        for blk in range(NBLK):
            sl = slice(blk * 512, (blk + 1) * 512)
            s_ = sq[blk % 2]
            ps = (psA, psB)[blk % 2]
            m.act(s_[:], xT[:, :, sl], AF.Square)
            for c in range(KC):
                m.mm(ps[:], self.ones_bf, s_[:, c, :], start=(c == 0), stop=(c == KC - 1))
            r = rs[blk % 2]
            m.act(r[:], ps[:], AF.Ln, bias=self.epsT[:], scale=1.0 / D)
            m.act(r[:], r[:], AF.Exp, scale=-0.5)
            self.dump("nm_r%d" % blk, r[:], [128, 512], F32)
            self.dump("nm_sq%d" % blk, s_[:], [128, KC, 512], BF16)
            for c in range(KC):
                t_ = tmp[ti % 3]
                ti += 1
                m.tt("dve", t_[:], xT[:, c, sl], r[:], ALU.mult)
                m.act(hT[:, c, sl], t_[:], AF.Identity, bias=self.mod[:, l, shbase + c, b:b + 1],
                      scale=self.nscale[:, l, j, c, b:b + 1])

    def attention(self, xT, b):
        m = self.m
        l = 0
        with ExitStack() as es:
            PS = [m.psum("aps%d" % i, [128, 512], F32, es) for i in range(8)]
            pA, pB = PS[0], PS[1]
            sTs = PS[2:4]
            accs = [(PS[4], PS[5]), (PS[6], PS[7])]
            hT = m.sbuf("a_hT", [128, KC, S], BF16, es)
            with ExitStack() as es2:
                self.norm_mod(xT, hT, l, 0, b, pA, pB, es2)
                m.barrier()
            self.dump("a_hT", hT[:], [128, KC, S], BF16)
            dvec0 = m.sbuf("a_vec0", [128, 4], F32, es)
            m.dma("sp", dvec0[:], self.da_vec_d[:])
            dvec = m.sbuf("a_vec", [128, 4], F32, es)
            m.ts("dve", dvec[:, 0:1], dvec0[:, 0:1], 0.125, None, ALU.mult)
            m.copy("dve", dvec[:, 1:2], dvec0[:, 1:2])
            m.ts("dve", dvec[:, 2:3], dvec0[:, 2:3], 1.0 - LAM_INIT0, None, ALU.mult)
            cb = m.sbuf("a_cb", [128, 8, 2], F32, es)
            m.dma("sp", cb[:], self.da_cb_d[:])
            lamv = m.sbuf("a_lamv", [128, 4, 64], F32, es)
            m.dma("sp", lamv[:], self.da_lam_d[:])
            lt = m.sbuf("a_lt", [128, 2, 64], F32, es)
            ls = m.sbuf("a_ls", [128, 2], F32, es)
            le = m.sbuf("a_le", [128, 2], F32, es)
            nlam = m.sbuf("a_nlam", [128, 1], F32, es)
            m.tt("dve", lt[:, 0, :], lamv[:, 0, :], lamv[:, 1, :], ALU.mult)
            m.tt("dve", lt[:, 1, :], lamv[:, 2, :], lamv[:, 3, :], ALU.mult)
            m.reduce("dve", ls[:], lt[:], ALU.add)
            m.act(le[:], ls[:], AF.Exp)
            m.stt("dve", nlam[:], le[:, 1:2], -LAM_INIT0, le[:, 0:1], ALU.add, ALU.subtract)

            wqkv = [m.sbuf("a_w%d" % i, [128, KC, 3, 128], BF16, es) for i in range(2)]
            wout = [m.sbuf("a_wo%d" % i, [128, D], BF16, es) for i in range(2)]
            Gs = [m.sbuf("a_G%d" % i, [128, 1152], F32, es) for i in range(2)]
            qn = [m.sbuf("a_qn%d" % i, [128, 2, S], BF16, es) for i in range(2)]
            for i in range(2):
                m.memset("pool", qn[i][64:128, 0, :], 0.0)
                m.memset("pool", qn[i][0:64, 1, :], 0.0)
            kn = [m.sbuf("a_kn%d" % i, [128, S], BF16, es) for i in range(2)]
            vh = [m.sbuf("a_v%d" % i, [128, NT, 128], BF16, es) for i in range(2)]
            sq = [m.sbuf("a_sq%d" % i, [128, 512], BF16, es) for i in range(2)]
            rs = [m.sbuf("a_rs%d" % i, [128, 512], F32, es) for i in range(2)]
            pT = [m.sbuf("a_pT%d" % i, [128, 512], BF16, es) for i in range(4)]
            rden = [m.sbuf("a_rd%d" % i, [128, 512], F32, es) for i in range(2)]
            t0 = [m.sbuf("a_t0%d" % i, [128, 512], F32, es) for i in range(2)]
            t1 = [m.sbuf("a_t1%d" % i, [128, 512], F32, es) for i in range(2)]
            ob = [m.sbuf("a_o%d" % i, [128, 512], F32, es) for i in range(2)]
            onT = [m.sbuf("a_on%d" % i, [128, 512], BF16, es) for i in range(2)]

            win = self.da_w_in_d.h.rearrange("(kc p) (t h n) -> p kc t h n", p=128, t=3, h=8)
            wo = self.da_w_out_d.h.rearrange("(h p) n -> p h n", p=128)
            cnt = {"pq": 0, "st": 0, "acc": 0, "pt": 0, "o": 0}

            def nxt(k):
                cnt[k] += 1
                return cnt[k] - 1

            def load_head(h):
                for t3 in range(3):
                    m.dma("pool", wqkv[h % 2][:, :, t3, :], self.da_w_in_d.v(win[:, :, t3, h, :]))
                m.dma("pool", wout[h % 2][:], self.da_w_out_d.v(wo[:, h, :]))
                m.dma("sp", Gs[h % 2][:], self.da_G_d[h])

            load_head(0)
            for h in range(8):
                w = wqkv[h % 2]
                wo_h = wout[h % 2]
                G = Gs[h % 2]
                if h + 1 < 8:
                    load_head(h + 1)
                q_h, k_h, v_h = qn[h % 2], kn[h % 2], vh[h % 2]
                items = [(which, blk) for which in (0, 1) for blk in range(NBLK)]

                def projA(ii):
                    which, blk = items[ii]
                    sl = slice(blk * 512, (blk + 1) * 512)
                    pp = PS[ii % 3]
                    for kc in range(KC):
                        m.mm(pp[:], w[:, kc, which, :], hT[:, kc, sl], start=(kc == 0), stop=(kc == KC - 1))
                    m.act(sq[ii % 2][:], pp[:], AF.Square)

                def projB(ii):
                    which, blk = items[ii]
                    sl = slice(blk * 512, (blk + 1) * 512)
                    pp = PS[ii % 3]
                    ssp = PS[3]
                    m.mm(ssp[:], self.bones_bf, sq[ii % 2][:])
                    r = rs[ii % 2]
                    m.act(r[:], ssp[:], AF.Ln, bias=self.epsT[:], scale=1.0 / 64)
                    m.act(r[:], r[:], AF.Exp, scale=-0.5)
                    if which == 1:
                        m.stt("dve", k_h[:, sl], pp[:], dvec[:, 1:2], r[:], ALU.mult, ALU.mult)
                    else:
                        m.stt("dve", q_h[0:64, 0, sl], pp[0:64, :], dvec[0:64, 0:1], r[0:64, :], ALU.mult, ALU.mult)
                        m.stt("dve", q_h[64:128, 1, sl], pp[64:128, :], dvec[64:128, 0:1], r[64:128, :],
                              ALU.mult, ALU.mult)

                for ii in range(len(items) + 1):
                    if ii < len(items):
                        projA(ii)
                    if ii >= 1:
                        projB(ii - 1)
                for g4 in range(4):
                    pp = PS[g4 % 2]
                    for j in range(4):
                        t = g4 * 4 + j
                        for kc in range(KC):
                            m.mm(pp[:, j * 128:(j + 1) * 128], hT[:, kc, t * 128:(t + 1) * 128], w[:, kc, 2, :],
                                 start=(kc == 0), stop=(kc == KC - 1))
                    m.copy("act", v_h[:, g4 * 4:(g4 + 1) * 4, :],
                           pp.v(pp.h[:, :].rearrange("p (j n) -> p j n", j=4)))
                tiles = [(qb, mp, kt) for qb in range(NBLK) for mp in range(2) for kt in range(NT)]
                NTL = len(tiles)
                LA = 2
                deferred = []
                sT3 = PS[1:4]

                def stageA(ti):
                    qb, mp, kt = tiles[ti]
                    rows = slice(mp * 64, (mp + 1) * 64)
                    qsl = slice(qb * 512, (qb + 1) * 512)
                    sT = sT3[ti % 3]
                    m.mm(sT[:], k_h[:, kt * 128:(kt + 1) * 128], q_h[:, mp, qsl])
                    p_ = pT[ti % 4]
                    dk = kt - 4 * qb
                    if dk <= -2:
                        m.act(p_[:], sT[:], AF.Exp, bias=cb[:, h, 0:1])
                    elif dk >= 5:
                        m.act(p_[:], sT[:], AF.Exp, bias=cb[:, h, 1:2])
                    else:
                        Dd = 128 * dk
                        m.tt("dve", sT[:], sT[:], G[:, 512 - Dd:1024 - Dd], ALU.add)
                        m.act(p_[:], sT[:], AF.Exp)

                def epilogue(ti, qb, mp, O, den, gi):
                    qsl = slice(qb * 512, (qb + 1) * 512)
                    par = qb % 2
                    rd = rden[mp]
                    m.act(rd[:], den[:], AF.Ln)
                    m.act(rd[:], rd[:], AF.Exp, scale=-1.0)
                    if mp == 0:
                        m.tt("dve", t0[par][:], O[:], rd[:], ALU.mult)
                        return
                    m.tt("dve", t1[par][:], O[:], rd[:], ALU.mult)
                    o_ = ob[par]
                    m.stt("dve", o_[:], t1[par][:], nlam[:], t0[par][:], ALU.mult, ALU.add)
                    if qb == 0:
                        self.dump("a_o", o_[:], [128, 512], F32)
                    s_ = sq[par]
                    m.act(s_[:], o_[:], AF.Square)
                    on_ = onT[par]
                    r = rs[par]

                    def e_ss():
                        m.mm(PS[0][:], self.ones_bf, s_[:])
                        m.act(r[:], PS[0][:], AF.Ln, bias=self.epsT[:], scale=1.0 / 128)
                        m.act(r[:], r[:], AF.Exp, scale=-0.5)
                        m.stt("dve", on_[:], o_[:], dvec[:, 2:3], r[:], ALU.mult, ALU.mult)
                    deferred.append((ti + 3, e_ss))
                    for oc in range(KC):
                        def e_op(oc=oc):
                            m.mm(PS[0][:], wo_h[:, oc * 128:(oc + 1) * 128], on_[:])
                            m.stt("dve", xT[:, oc, qsl], PS[0][:], self.mod[:, l, 16 + oc, b:b + 1], xT[:, oc, qsl],
                                  ALU.mult, ALU.add)
                        deferred.append((ti + 7 + 2 * oc, e_op))

                def stageB(ti):
                    qb, mp, kt = tiles[ti]
                    gi = qb * 2 + mp
                    O, den = accs[gi % 2]
                    p_ = pT[ti % 4]
                    m.mm(O[:], v_h[:, kt, :], p_[:], start=(kt == 0), stop=(kt == NT - 1))
                    m.mm(den[:], self.ones_bf, p_[:], start=(kt == 0), stop=(kt == NT - 1))
                    if kt == NT - 1:
                        epilogue(ti, qb, mp, O, den, gi)

                for step in range(NTL + LA):
                    if step < NTL:
                        stageA(step)
                    if step >= LA:
                        stageB(step - LA)
                    while deferred and deferred[0][0] <= step - LA:
                        deferred.pop(0)[1]()
                while deferred:
                    deferred.pop(0)[1]()
            m.barrier()

    def moe(self, xT, b, l):
        m = self.m
        with ExitStack() as es:
            PS = [m.psum("mps%d" % i, [128, 512], F32, es) for i in range(8)]
            hT = m.sbuf("m_hT", [128, KC, S], BF16, es)
            with ExitStack() as es2:
                self.norm_mod(xT, hT, l, 1, b, PS[0], PS[1], es2)
                m.barrier()
            with ExitStack() as es2:
                wr = m.sbuf("m_wr", [128, KC, 36], BF16, es2)
                m.dma("pool", wr[:], self.moe_wr_d.v(self.moe_wr_d.h[l].rearrange("(kc p) n -> p kc n", p=128)))
                gT = m.sbuf("m_gT", [32, S], F32, es2)
                BIG = 30000.0

                def sb(name, shape):
                    return m.sbuf(name, shape, F32, es2)
                gl, el, gmx, gsh, gex, gsum, pen, em, m12, msk, em2, esh, ee, dd, e2, den, rr, gates = (
                    sb("r_gl", [128, NT, 4]), sb("r_el", [128, NT, 32]), sb("r_gmx", [128, NT]),
                    sb("r_gsh", [128, NT, 4]), sb("r_gex", [128, NT, 4]), sb("r_gsum", [128, NT]),
                    sb("r_pen", [128, NT, 4]), sb("r_em", [128, NT, 32]), sb("r_m12", [128, 2, NT]),
                    sb("r_msk", [128, NT, 32]), sb("r_em2", [128, NT, 32]), sb("r_esh", [128, NT, 32]),
                    sb("r_ee", [128, NT, 32]), sb("r_dd", [128, NT]), sb("r_e2", [128, NT]),
                    sb("r_den", [128, NT]), sb("r_rr", [128, NT]), sb("r_gates", [128, NT, 32]))

                def bc(t, n):
                    return V(bass.AP(t.h, 0, [[t.h[:].ap[0][0], 128], [1, NT], [0, n]]), (t.buf,))

                for half in range(2):
                    ps = PS[half]
                    for t8 in range(8):
                        t = half * 8 + t8
                        for kc in range(KC):
                            m.mm(ps[:, t8 * 36:(t8 + 1) * 36], hT[:, kc, t * 128:(t + 1) * 128], wr[:, kc, :],
                                 start=(kc == 0), stop=(kc == KC - 1))
                    ps3 = ps.v(ps.h[:, 0:288].rearrange("p (t c) -> p t c", t=8))
                    m.copy("act", gl[:, half * 8:(half + 1) * 8, :], ps3[:, :, 0:4])
                    m.copy("act", el[:, half * 8:(half + 1) * 8, :], ps3[:, :, 4:36])
                m.reduce("dve", gmx[:], gl[:], ALU.max)
                m.tt("dve", gsh[:], gl[:], bc(gmx, 4), ALU.subtract)
                m.act(gex[:], gsh[:], AF.Exp)
                m.reduce("dve", gsum[:], gex[:], ALU.add)
                m.ts("dve", pen[:], gsh[:], 0.0, BIG, ALU.is_ge, ALU.mult)
                m.ts("dve", pen[:], pen[:], -BIG, None, ALU.add)
                pen_bc = V(bass.AP(pen.h, 0, [[pen.h[:].ap[0][0], 128], [1, NT * 4], [0, 8]]), (pen.buf,))
                m.tt("dve", em.v(em.h[:].rearrange("p t (g e) -> p (t g) e", g=4)),
                     el.v(el.h[:].rearrange("p t (g e) -> p (t g) e", g=4)), pen_bc, ALU.add)
                m.reduce("dve", m12[:, 0, :], em[:], ALU.max)
                m.tt("dve", msk[:], em[:], bc(T(m12.h, "x", m12.buf), 32), ALU.is_ge)
                m.stt("dve", em2[:], msk[:], -BIG, em[:], ALU.mult, ALU.add)
                m.reduce("dve", m12[:, 1, :], em2[:], ALU.max)
                m2_bc = V(bass.AP(m12.h, NT, [[m12.h[:].ap[0][0], 128], [1, NT], [0, 32]]), (m12.buf,))
                m.tt("dve", msk[:], em[:], m2_bc, ALU.is_ge)
                m.tt("dve", esh[:], em[:], bc(T(m12.h, "x", m12.buf), 32), ALU.subtract)
                m.act(ee[:], esh[:], AF.Exp)
                m.tt("dve", dd[:], m12[:, 1, :], m12[:, 0, :], ALU.subtract)
                m.act(e2[:], dd[:], AF.Exp)
                m.stt("dve", den[:], e2[:], 1.0, gsum[:], ALU.add, ALU.mult)
                m.recip(rr[:], den[:])
                m.tt("dve", gates[:], ee[:], bc(rr, 32), ALU.mult)
                m.tt("dve", gates[:], gates[:], msk[:], ALU.mult)
                for g4 in range(4):
                    pst = PS[2 + g4 % 2]
                    for j in range(4):
                        t = g4 * 4 + j
                        m.transpose(pst[0:32, j * 128:(j + 1) * 128], gates[:, t, :], self.ident)
                    m.copy("act", gT[:, g4 * 512:(g4 + 1) * 512], pst[0:32, :])
                m.dma("sp", self.gT_d[:], gT[:])
                m.barrier()
            w1b = [m.sbuf("m_w1%d" % i, [128, KC, 2, 256], BF16, es) for i in range(2)]
            w3b = [m.sbuf("m_w3%d" % i, [128, KC, 2, 256], BF16, es) for i in range(2)]
            w2b = [m.sbuf("m_w2%d" % i, [128, 2, 2, D], BF16, es) for i in range(2)]
            gbc = [m.sbuf("m_gbc%d" % i, [128, 2, 512], F32, es) for i in range(2)]
            sil = [m.sbuf("m_sil%d" % i, [128, 512], F32, es) for i in range(2)]
            tt_ = [m.sbuf("m_tt%d" % i, [128, 512], F32, es) for i in range(2)]
            aT = [m.sbuf("m_aT%d" % i, [128, 4, 512], BF16, es) for i in range(2)]
            w1v = self.moe_w1_d.h[l].rearrange("e (kc p) n -> p kc e n", p=128)
            w3v = self.moe_w3_d.h[l].rearrange("e (kc p) n -> p kc e n", p=128)
            w2v = self.moe_w2_d.h[l].rearrange("e (hc p) n -> p e hc n", p=128)
            gtv = self.gT_d.h
            hp = [PS[0], PS[1], PS[2], PS[3]]
            yp = [PS[4], PS[5], PS[6], PS[7]]
            ci = 0
            yi = 0
            gi = 0
            def load_w(ep):
                w1, w3, w2 = w1b[ep % 2], w3b[ep % 2], w2b[ep % 2]
                for e_ in range(2):
                    m.dma("pool", w1[:, :, e_, :], self.moe_w1_d.v(w1v[:, :, 2 * ep + e_, :]))
                    m.dma("pool", w3[:, :, e_, :], self.moe_w3_d.v(w3v[:, :, 2 * ep + e_, :]))
                    m.dma("pool", w2[:, e_, :, :], self.moe_w2_d.v(w2v[:, 2 * ep + e_, :, :]))

            def load_g(it):
                ep, sbk = it // NBLK, it % NBLK
                src = bass.AP(gtv.tensor, 2 * ep * S + sbk * 512, [[0, 128], [S, 2], [1, 512]])
                m.dma("sp", gbc[it % 2][:], self.gT_d.v(src))

            ctr = {"ci": 0, "yi": 0}

            def Hpart(it):
                ep, sbk = it // NBLK, it % NBLK
                w1, w3 = w1b[ep % 2], w3b[ep % 2]
                sl = slice(sbk * 512, (sbk + 1) * 512)
                g_ = gbc[it % 2]
                a_ = aT[it % 2]
                for hc in range(4):
                    e2_, hh = hc // 2, hc % 2
                    ci = ctr["ci"]
                    h1p, h3p = hp[(ci % 2) * 2], hp[(ci % 2) * 2 + 1]
                    for kc in range(KC):
                        m.mm(h1p[:], w1[:, kc, e2_, hh * 128:(hh + 1) * 128], hT[:, kc, sl],
                             start=(kc == 0), stop=(kc == KC - 1))
                    for kc in range(KC):
                        m.mm(h3p[:], w3[:, kc, e2_, hh * 128:(hh + 1) * 128], hT[:, kc, sl],
                             start=(kc == 0), stop=(kc == KC - 1))
                    s_ = sil[ci % 2]
                    t_ = tt_[ci % 2]
                    ctr["ci"] += 1
                    m.act(s_[:], h1p[:], AF.Silu)
                    m.tt("dve", t_[:], h3p[:], s_[:], ALU.mult)
                    m.tt("pool", a_[:, hc, :], t_[:], g_[:, e2_, :], ALU.mult)

            def Ypart(it):
                ep, sbk = it // NBLK, it % NBLK
                w2 = w2b[ep % 2]
                sl = slice(sbk * 512, (sbk + 1) * 512)
                a_ = aT[it % 2]
                for oc in range(KC):
                    y_ = yp[ctr["yi"] % 4]
                    ctr["yi"] += 1
                    for hc in range(4):
                        m.mm(y_[:], w2[:, hc // 2, hc % 2, oc * 128:(oc + 1) * 128], a_[:, hc, :],
                             start=(hc == 0), stop=(hc == 3))
                    m.stt("dve", xT[:, oc, sl], y_[:], self.mod[:, l, 40 + oc, b:b + 1], xT[:, oc, sl],
                          ALU.mult, ALU.add)

            NIT = 16 * NBLK
            load_w(0)
            load_g(0)
            load_g(1)
            Hpart(0)
            for it in range(NIT):
                ep, sbk = it // NBLK, it % NBLK
                if sbk == 0 and ep + 1 < 16:
                    load_w(ep + 1)
                if it + 2 < NIT:
                    load_g(it + 2)
                if it + 1 < NIT:
                    Hpart(it + 1)
                Ypart(it)
            m.barrier()

    def mlstm(self, xT, b):
        m = self.m
        nc = self.nc
        l = 1
        ISQ = 512 ** -0.5
        with ExitStack() as es:
            dCps = m.psum("l_dC", [128, 4, 512], F32, es)
            PS4 = m.psum("l_ps4", [128, 512], F32, es)
            PS5 = m.psum("l_ps5", [128, 512], F32, es)
            PS6 = m.psum("l_ps6", [128, 4, 128], BF16, es)
            PS7 = m.psum("l_ps7", [128, 512], F32, es)
            banks = [T(dCps.h, "dCb%d" % j) for j in range(4)]

            def bank(j):
                return banks[j].v(dCps.h[:, j, :])

            lvec = m.sbuf("l_vec", [128, 16, 8], F32, es)
            m.dma("sp", lvec[:], self.ml_vec_d[:])
            gb = m.sbuf("l_gb", [128, 16], F32, es)
            m.dma("sp", gb[:], self.ml_gb_d[:])
            pre = m.sbuf("l_pre", [128, 16, 2, 8], F32, es)
            g_all = m.sbuf("l_g", [128, 16, 2, 4], F32, es)
            dec_all = m.sbuf("l_dec", [128, 16, 2, 4], F32, es)
            fl_all = m.sbuf("l_fl", [128, 16, 2, 4], F32, es)
            xc_s = [T(self.ml_xc_d.h, "xc_s%d" % i) for i in range(16)]
            xm_s = [T(self.ml_xm_d.h, "xm_s%d" % i) for i in range(16)]
            zs_s = [T(self.ml_zs_d.h, "zs_s%d" % i) for i in range(16)]
            with ExitStack() as es1:
                hT = m.sbuf("l_hT", [128, KC, S], BF16, es1)
                with ExitStack() as es2:
                    self.norm_mod(xT, hT, l, 0, b, PS4, PS5, es2)
                    m.barrier()
                bd = m.sbuf("l_bd", [128, 3, 16, 128], BF16, es1)
                gw = m.sbuf("l_gw", [128, 3, 16, 16], BF16, es1)
                for src in range(3):
                    m.dma("pool", bd[:, src, :, :], self.ml_bd_d[:, src, :, :])
                    m.dma("pool", gw[:, src, :, :], self.ml_gw_d[:, src, :, :])
                pre_ps = PS7.v(PS7.h[:, 0:256].rearrange("p (t c) -> p t c", t=16))
                m.memset("dve", PS7[:, 0:256], 0.0)
                wb = [m.sbuf("l_w%d" % i, [128, KC, 2, 128], BF16, es1) for i in range(2)]
                xm = [m.sbuf("l_xm%d" % i, [128, S + 4], F32, es1) for i in range(2)]
                cv = [m.sbuf("l_cv%d" % i, [128, S], F32, es1) for i in range(2)]
                zs = [m.sbuf("l_zs%d" % i, [128, S], BF16, es1) for i in range(2)]
                xc = [m.sbuf("l_xc%d" % i, [128, S], BF16, es1) for i in range(2)]
                xmb = [m.sbuf("l_xmb%d" % i, [128, S], BF16, es1) for i in range(2)]
                qkv = [m.sbuf("l_qkv%d" % i, [128, 3, S], BF16, es1) for i in range(2)]
                for i in range(2):
                    m.memset("dve", xm[i][:, 0:2], 0.0)
                    m.memset("dve", xm[i][:, S + 2:S + 4], 0.0)
                winv = self.ml_w_in_d.h.rearrange("(kc p) n -> p kc n", p=128)

                def load_w(fc):
                    m.dma("pool", wb[fc % 2][:, :, 0, :], self.ml_w_in_d.v(winv[:, :, fc * 128:(fc + 1) * 128]))
                    m.dma("pool", wb[fc % 2][:, :, 1, :],
                          self.ml_w_in_d.v(winv[:, :, 2048 + fc * 128:2048 + (fc + 1) * 128]))

                load_w(0)
                load_w(1)
                pctr = [0]

                def inproj(fc):
                    w = wb[fc % 2]
                    xm_, zs_ = xm[fc % 2], zs[fc % 2]
                    for blk in range(NBLK):
                        sl = slice(blk * 512, (blk + 1) * 512)
                        p1 = bank(pctr[0] % 4)
                        pctr[0] += 1
                        for kc in range(KC):
                            m.mm(p1, w[:, kc, 0, :], hT[:, kc, sl], start=(kc == 0), stop=(kc == KC - 1))
                        m.copy("act", xm_[:, 2 + blk * 512:2 + (blk + 1) * 512], p1)
                        p2 = bank(pctr[0] % 4)
                        pctr[0] += 1
                        for kc in range(KC):
                            m.mm(p2, w[:, kc, 1, :], hT[:, kc, sl], start=(kc == 0), stop=(kc == KC - 1))
                        m.act(zs_[:, sl], p2, AF.Silu)
                    if fc + 2 < 16:
                        load_w(fc + 2)

                inproj(0)
                pi = 0
                for fc in range(16):
                    if fc + 1 < 16:
                        inproj(fc + 1)
                    xm_, cv_, zs_, xc_, xmb_, qkv_ = (xm[fc % 2], cv[fc % 2], zs[fc % 2], xc[fc % 2], xmb[fc % 2],
                                                      qkv[fc % 2])
                    for blk in range(0):
                        sl = slice(blk * 512, (blk + 1) * 512)
                        p1 = bank(pi % 4)
                        pi += 1
                        for kc in range(KC):
                            m.mm(p1, w[:, kc, 0, :], hT[:, kc, sl], start=(kc == 0), stop=(kc == KC - 1))
                        m.copy("act", xm_[:, 2 + blk * 512:2 + (blk + 1) * 512], p1)
                        p2 = bank(pi % 4)
                        pi += 1
                        for kc in range(KC):
                            m.mm(p2, w[:, kc, 1, :], hT[:, kc, sl], start=(kc == 0), stop=(kc == KC - 1))
                        m.act(zs_[:, sl], p2, AF.Silu)
                    m.ts("dve", cv_[:], xm_[:, 0:S], lvec[:, fc, 0:1], None, ALU.mult)
                    for j in range(1, 5):
                        m.stt("dve", cv_[:], xm_[:, j:j + S], lvec[:, fc, j:j + 1], cv_[:], ALU.mult, ALU.add)
                    m.act(xc_[:], cv_[:], AF.Silu, bias=lvec[:, fc, 5:6])
                    m.copy("pool", xmb_[:], xm_[:, 2:2 + S])
                    m.dma("sp", zs_s[fc].v(self.ml_zs_d.h[fc]), zs_[:])
                    m.dma("sp", xc_s[fc].v(self.ml_xc_d.h[fc]), xc_[:])
                    m.dma("sp", xm_s[fc].v(self.ml_xm_d.h[fc]), xmb_[:])
                    for blk in range(NBLK):
                        sl = slice(blk * 512, (blk + 1) * 512)
                        for src in range(3):
                            p1 = bank(pctr[0] % 4)
                            pctr[0] += 1
                            rhs = xmb_[:, sl] if src == 2 else xc_[:, sl]
                            m.mm(p1, bd[:, src, fc, :], rhs)
                            if src == 1:
                                m.copy("dve", qkv_[:, src, sl], p1)
                            else:
                                m.copy("act", qkv_[:, src, sl], p1)
                    for t in range(NT):
                        for src in range(3):
                            m.emit("pe", lambda t=t, src=src, qkv_=qkv_: nc.tensor.matmul(
                                pre_ps.ap[:, t, :], qkv_.h[:, src, t * 128:(t + 1) * 128], gw.h[:, src, fc, :],
                                start=False, stop=False, skip_group_check=True),
                                [qkv_[:], gw[:]], [PS7[:]])
                gb_bc = V(bass.AP(gb.h, 0, [[gb.h[:].ap[0][0], 128], [0, 16], [1, 16]]), (gb.buf,))
                m.tt("dve", pre.v(pre.h[:].rearrange("p t d g -> p t (d g)")), pre_ps, gb_bc, ALU.add)
                self.dump("l_pre", pre[:], [128, 16, 2, 8], F32)
                m.barrier()
            with ExitStack() as es1:
                def sbt(name, shape=(128, 16, 2, 4)):
                    return m.sbuf(name, list(shape), F32, es1)
                ex, lp, u, nb, ubc, a_all, darg, garg, farg = (sbt("g_ex"), sbt("g_lp"), sbt("g_u"), sbt("g_nb"),
                                                                sbt("g_ubc"), sbt("g_a"), sbt("g_darg"),
                                                                sbt("g_garg"), sbt("g_farg"))
                nbL = sbt("g_nbL")
                umax = sbt("g_umax", (128, 1))
                dg = sbt("g_dg", (128, 128))
                m_all = sbt("g_m", (128, 17, 2, 4))
                fcols = pre[:, :, :, 4:8]
                icols = pre[:, :, :, 0:4]
                m.act(ex[:], fcols, AF.Exp, scale=-1.0)
                m.act(lp[:], ex[:], AF.Ln, bias=self.ones_f[:, 0:1])
                lp2 = lp.v(lp.h[:].rearrange("p t d h -> p (t d h)"))
                pf, pb, pl, pt_, pu = bank(0), bank(1), bank(2), bank(3), PS4[:, 0:128]
                m.mm(banks[0].v(dCps.h[:, 0, 0:128]), self.cst[:, 3, :], lp2)
                m.mm(banks[1].v(dCps.h[:, 1, 0:128]), self.cst[:, 4, :], lp2)
                m.mm(banks[2].v(dCps.h[:, 2, 0:128]), self.ones_f, lp2)

                def v4(bk, j):
                    return bk.v(dCps.h[:, j, 0:128].rearrange("p (t d h) -> p t d h", t=16, d=2))
                m.copy("dve", nb[:, :, 0, :], V(v4(banks[0], 0).ap[:, :, 0, :], (banks[0].buf,)))
                m.copy("dve", nb[:, :, 1, :], V(v4(banks[1], 1).ap[:, :, 1, :], (banks[1].buf,)))
                m.copy("dve", nbL[:], v4(banks[2], 2))
                m.tt("dve", u[:], icols, nb[:], ALU.add)
                u2 = u.v(u.h[:].rearrange("p t d h -> p (t d h)"))
                m.transpose(banks[3].v(dCps.h[:, 3, 0:128]), u2, self.ident)
                m.reduce("dve", umax[:], banks[3].v(dCps.h[:, 3, 0:128]), ALU.max)
                m.ts("dve", dg[:], self.ident, umax[:], None, ALU.mult)
                m.mm(pu, self.ones_f, dg[:])
                m.copy("dve", ubc.v(ubc.h[:].rearrange("p t d h -> p (t d h)")), pu)
                m.memset("dve", m_all[:, 0, 0, :], 0.0)
                m.memset("dve", m_all[:, 16, 1, :], 0.0)
                for st in range(16):
                    for d_ in range(2):
                        c = st if d_ == 0 else 15 - st
                        src_i = c if d_ == 0 else c + 1
                        dst_i = c + 1 if d_ == 0 else c
                        m.tt("dve", a_all[:, c, d_, :], m_all[:, src_i, d_, :], ubc[:, c, d_, :], ALU.max)
                        m.tt("dve", darg[:, c, d_, :], m_all[:, src_i, d_, :], a_all[:, c, d_, :], ALU.subtract)
                        m.tt("dve", m_all[:, dst_i, d_, :], a_all[:, c, d_, :], nbL[:, c, d_, :], ALU.subtract)
                m.tt("dve", garg[:], u[:], a_all[:], ALU.subtract)
                m.tt("dve", farg[:], nb[:], a_all[:], ALU.subtract)
                m.act(g_all[:], garg[:], AF.Exp)
                m.act(dec_all[:], darg[:], AF.Exp)
                m.act(fl_all[:], farg[:], AF.Exp)
                self.dump("l_g", g_all[:], [128, 16, 2, 4], F32)
                self.dump("l_dec", dec_all[:], [128, 16, 2, 4], F32)
                self.dump("l_fl", fl_all[:], [128, 16, 2, 4], F32)
                m.barrier()
            wov = self.ml_w_out_d.h.rearrange("(c p) n -> p c n", p=128)
            bdh = m.sbuf("l_bdh", [128, 3, 4, 128], BF16, es)
            wo = [m.sbuf("l_wo%d" % i, [128, 4, D], BF16, es) for i in range(1)]
            qT = m.sbuf("l_qT", [128, 4, S], BF16, es)
            kT = m.sbuf("l_kT", [128, 4, S], BF16, es)
            ktm = m.sbuf("l_ktm", [128, NT, 512], BF16, es)
            vtm = m.sbuf("l_vtm", [128, NT, 512], BF16, es)
            hfw = m.sbuf("l_hfw", [128, NT, 512], BF16, es)
            Cst = m.sbuf("l_C", [128, 4, 512], F32, es)
            Cbs = [m.sbuf("l_Cb%d" % i, [128, 4, 512], BF16, es) for i in range(3)]
            nst = m.sbuf("l_n", [128, 4], F32, es)
            nbfs = [m.sbuf("l_nb%d" % i, [128, 4], BF16, es) for i in range(3)]
            xin = [m.sbuf("l_xin%d" % i, [128, 4, 512], BF16, es) for i in range(1)]
            xmin = [m.sbuf("l_xmin%d" % i, [128, 4, 512], BF16, es) for i in range(1)]
            xce = [m.sbuf("l_xce%d" % i, [128, 4, 128], BF16, es) for i in range(2)]
            zse = [m.sbuf("l_zse%d" % i, [128, 4, 128], BF16, es) for i in range(2)]
            PT = [m.sbuf("l_PT%d" % i, [128, 128], BF16, es) for i in range(2)]
            qp = [m.sbuf("l_qp%d" % i, [128, 4, 128], BF16, es) for i in range(2)]
            kw = [m.sbuf("l_kw%d" % i, [128, 512], BF16, es) for i in range(2)]
            rd = [m.sbuf("l_rd%d" % i, [128, 1], F32, es) for i in range(2)]
            ss = [m.sbuf("l_ss%d" % i, [128, 1], F32, es) for i in range(2)]
            t1 = [m.sbuf("l_t1%d" % i, [128, 128], F32, es) for i in range(2)]
            t2 = [m.sbuf("l_t2%d" % i, [128, 4, 128], F32, es) for i in range(1)]
            yT = [m.sbuf("l_yT%d" % i, [128, 4, 512], BF16, es) for i in range(1)]
            sT = PS7[:, 0:128]
            den = PS5[:, 0:1]
            dn = PS5[:, 8:12]
            ones_col = self.ones_bf
            xib = [xin[0], yT[0]]
            xmb2 = [xmin[0], Cbs[0]]

            def load_bd(h):
                for src in range(3):
                    m.dma("pool", bdh[:, src, :, :], self.ml_bd_d[:, src, 4 * h:4 * h + 4, :])

            load_bd(0)
            for h in range(4):
                m.dma("pool", wo[0][:], self.ml_w_out_d.v(wov[:, 4 * h:4 * h + 4, :]))

                def load_blk(blk):
                    sl_ = slice(blk * 512, (blk + 1) * 512)
                    for j in range(4):
                        fc = 4 * h + j
                        m.dma("sp", xib[blk % 2][:, j, :], xc_s[fc].v(self.ml_xc_d.h[fc][:, sl_]))
                        m.dma("sp", xmb2[blk % 2][:, j, :], xm_s[fc].v(self.ml_xm_d.h[fc][:, sl_]))

                pj = 0
                load_blk(0)
                for blk in range(NBLK):
                    sl = slice(blk * 512, (blk + 1) * 512)
                    if blk + 1 < NBLK:
                        load_blk(blk + 1)
                    xi, xmi = xib[blk % 2], xmb2[blk % 2]
                    for j in range(4):
                        fc = 4 * h + j
                        p1 = (PS4, PS5)[pj % 2]
                        pj += 1
                        m.mm(p1[:], bdh[:, 0, j, :], xi[:, j, :])
                        m.copy("dve" if j % 2 else "act", qT[:, j, sl], p1[:])
                        p1 = (PS4, PS5)[pj % 2]
                        pj += 1
                        m.mm(p1[:], bdh[:, 1, j, :], xi[:, j, :])
                        m.act(kT[:, j, sl], p1[:], AF.Copy, scale=ISQ)
                    for tt4 in range(4):
                        t = blk * 4 + tt4
                        tl = slice(tt4 * 128, (tt4 + 1) * 128)
                        p1 = (PS4, PS5)[pj % 2]
                        pj += 1
                        for j in range(4):
                            m.mm(p1[:, j * 128:(j + 1) * 128], xi[:, j, tl], bdh[:, 1, j, :])
                        m.ts("dve", ktm[:, t, :], p1[:], ISQ, None, ALU.mult)
                        p1 = (PS4, PS5)[pj % 2]
                        pj += 1
                        for j in range(4):
                            m.mm(p1[:, j * 128:(j + 1) * 128], xmi[:, j, tl], bdh[:, 2, j, :])
                        m.copy("dve", vtm[:, t, :], p1[:])
                if h + 1 < 4:
                    load_bd(h + 1)
                for d_ in range(2):
                    m.memset("dve", Cst[:], 0.0)
                    m.memset("pool", Cbs[2][:], 0.0)
                    m.memset("dve", nst[:], 0.0)
                    m.memset("pool", nbfs[2][:], 0.0)
                    mask = self.cst[:, 3 + d_, :]

                    def chunk(st):
                        return st if d_ == 0 else 15 - st

                    def partA(st):
                        c = chunk(st)
                        tsl = slice(c * 128, (c + 1) * 128)
                        gcol = g_all[:, c, d_, h:h + 1]
                        dcol = dec_all[:, c, d_, h:h + 1]
                        for j in range(4):
                            m.mm(sT, kT[:, j, tsl], qT[:, j, tsl], start=(j == 0), stop=(j == 3))
                        P_ = PT[st % 2]
                        m.stt("dve", P_[:], sT, gcol, mask, ALU.mult, ALU.mult)
                        k_ = kw[st % 2]
                        m.ts("dve", k_[:], ktm[:, c, :], gcol, None, ALU.mult)
                        q_ = qp[st % 2]
                        m.act(q_[:], qT[:, :, tsl], AF.Copy, scale=dcol)
                        for j in range(4):
                            m.mm(V(dn.ap[:, j:j + 1], dn.bufs), k_[:, j * 128:(j + 1) * 128], ones_col[:, 0:1])
                        for j in range(4):
                            m.mm(dCps[:, j, :], k_[:, j * 128:(j + 1) * 128], vtm[:, c, :])
                        m.stt("dve", nst[:], nst[:], dcol, dn, ALU.mult, ALU.add)
                        m.copy("dve", nbfs[st % 3][:], nst[:])
                        m.stt("dve", Cst[:], Cst[:], dcol, dCps[:], ALU.mult, ALU.add)

                    def partB(st):
                        c = chunk(st)
                        fcol = fl_all[:, c, d_, h:h + 1]
                        Cb = Cbs[(st + 2) % 3]
                        nbf = nbfs[(st + 2) % 3]
                        P_ = PT[st % 2]
                        q_ = qp[st % 2]
                        m.mm(PS4[:], P_[:], vtm[:, c, :], start=True, stop=False)
                        for j in range(4):
                            m.mm(PS4[:], q_[:, j, :], Cb[:, j, :], start=False, stop=(j == 3))
                        m.mm(den, P_[:], ones_col[:, 0:1], start=True, stop=False)
                        for j in range(4):
                            m.mm(den, q_[:, j, :], nbf[:, j:j + 1], start=False, stop=(j == 3))
                        r_ = rd[st % 2]
                        m.act(r_[:], den, AF.Abs)
                        m.ts("dve", r_[:], r_[:], fcol, None, ALU.max)
                        m.recip(r_[:], r_[:])
                        if d_ == 0:
                            m.act(hfw[:, c, :], PS4[:], AF.Copy, scale=r_[:])
                        else:
                            m.stt("dve", hfw[:, c, :], PS4[:], r_[:], hfw[:, c, :], ALU.mult, ALU.add)

                    partA(0)
                    m.copy("act", Cbs[0][:], Cst[:])
                    for st in range(16):
                        if st + 1 < 16:
                            partA(st + 1)
                        partB(st)
                        if st + 1 < 16:
                            m.copy("act", Cbs[(st + 1) % 3][:], Cst[:])
                hnb = [xmin[0][:, 1, :], xmin[0][:, 0, :]]
                yTb = [yT[0], xin[0]]

                def E1(c):
                    tsl = slice(c * 128, (c + 1) * 128)
                    for j in range(4):
                        fc = 4 * h + j
                        m.dma("sp", xce[c % 2][:, j, :], xc_s[fc].v(self.ml_xc_d.h[fc][:, tsl]))
                        m.dma("sp", zse[c % 2][:, j, :], zs_s[fc].v(self.ml_zs_d.h[fc][:, tsl]))
                    s_ = ss[c % 2]
                    hn_ = hnb[c % 2]
                    m.act(hn_, hfw[:, c, :], AF.Square, accum_out=s_[:])
                    m.act(s_[:], s_[:], AF.Ln, bias=self.epsT[:], scale=1.0 / 512)
                    m.act(s_[:], s_[:], AF.Exp, scale=-0.5)
                    m.act(hn_, hfw[:, c, :], AF.Copy, scale=s_[:])

                def E2(c):
                    hn_ = hnb[c % 2]
                    for j in range(4):
                        m.transpose(PS6[:, j, :], hn_[:, j * 128:(j + 1) * 128], self.ident_bf)
                    t1_, t2_ = t1[0], t2[0]
                    y_ = yTb[(c // 4) % 2]
                    csl = slice((c % 4) * 128, (c % 4 + 1) * 128)
                    for j in range(4):
                        fc = 4 * h + j
                        m.act(t1[j % 2][:], PS6[:, j, :], AF.Copy, scale=lvec[:, fc, 7:8])
                        m.stt("dve", t2_[:, j, :], xce[c % 2][:, j, :], lvec[:, fc, 6:7], t1[j % 2][:],
                              ALU.mult, ALU.add)
                    m.tt("dve", V(y_.h[:, :, csl], (y_.buf,)), t2_[:], zse[c % 2][:], ALU.mult)
                    if c % 4 == 3:
                        blk = c // 4
                        bsl = slice(blk * 512, (blk + 1) * 512)
                        for oc in range(KC):
                            pp = (PS5, PS4)[oc % 2]
                            for j in range(4):
                                m.mm(pp[:], wo[0][:, j, oc * 128:(oc + 1) * 128], y_[:, j, :],
                                     start=(j == 0), stop=(j == 3))
                            m.stt("dve", xT[:, oc, bsl], pp[:], self.mod[:, l, 16 + oc, b:b + 1],
                                  xT[:, oc, bsl], ALU.mult, ALU.add)

                for stp in range(17):
                    if stp < 16:
                        E1(stp)
                    if stp >= 1:
                        E2(stp - 1)
            m.barrier()

    def build(self, stages=("attn", "moe0", "mlstm", "moe1")):
        m = self.m
        self.declare(stages)
        self.consts()
        self.prologue()
        for b in range(NB):
            with ExitStack() as es:
                xT = m.sbuf("xT", [128, KC, S], F32, es)
                with ExitStack() as es2:
                    PS = [m.psum("iops%d" % i, [128, 512], F32, es2) for i in range(4)]
                    self.load_x(b, xT, PS)
                if "attn" in stages:
                    self.attention(xT, b)
                if "moe0" in stages:
                    self.moe(xT, b, 0)
                if "mlstm" in stages:
                    self.mlstm(xT, b)
                if "moe1" in stages:
                    self.moe(xT, b, 1)
                with ExitStack() as es2:
                    PS = [m.psum("iops%d" % i, [128, 512], F32, es2) for i in range(4)]
                    self.store_x(b, xT, PS)
        m.finish()
        self.es.close()
        return self.nc


def _fm(v, nchunk):
    return np.ascontiguousarray(np.asarray(v, np.float32).reshape(nchunk, 128).T)


def host_layout(inp, core):
    f32 = np.float32
    d = {}
    bs = slice(core * NB, (core + 1) * NB)
    d["x"] = np.ascontiguousarray(inp["x"][bs])
    c = np.asarray(inp["c"][bs], f32)
    d["cT"] = np.ascontiguousarray(c.T.reshape(KC, 128, NB).transpose(1, 0, 2))
    d["ada_w"] = inp["ada_w"]
    d["ada_bT"] = np.ascontiguousarray(np.asarray(inp["ada_b"], f32).reshape(2, 48, 128).transpose(2, 0, 1))
    ng = np.stack([inp["norm_mix_g"], inp["norm_ffn_g"]], axis=1)
    d["normgT"] = np.ascontiguousarray(np.asarray(ng, f32).reshape(2, 2, KC, 128).transpose(3, 0, 1, 2))
    cst = np.zeros((128, 6, 128), f32)
    cst[:, 0, :] = np.eye(128)
    cst[:, 1, :] = 1.0
    cst[0:64, 2, 0:64] = 1.0
    cst[64:128, 2, 64:128] = 1.0
    ii = np.arange(128)
    cst[:, 3, :] = (ii[:, None] <= ii[None, :])
    cst[:, 4, :] = (ii[:, None] >= ii[None, :])
    d["consts"] = cst
    d["da_w_in"] = inp["da_w_in"][0]
    d["da_w_out"] = inp["da_w_out"][0]
    dv = np.zeros((128, 4), f32)
    dv[:, 0] = np.tile(inp["da_q_gain"][0], 2)
    dv[:, 1] = np.tile(inp["da_k_gain"][0], 2)
    dv[:, 2] = inp["da_subln_g"][0]
    d["da_vec"] = dv
    lam = np.stack([inp["da_lam_q1"][0], inp["da_lam_k1"][0], inp["da_lam_q2"][0], inp["da_lam_k2"][0]])
    d["da_lam"] = np.ascontiguousarray(np.broadcast_to(lam[None], (128, 4, 64)).astype(f32))
    i_ = np.arange(128)[:, None]
    c_ = np.arange(1152)[None, :]
    idx = _t5_bucket(i_ - c_ + 512)
    tab = np.asarray(inp["rel_table"], f32)
    d["da_G"] = np.ascontiguousarray(tab[idx].transpose(2, 0, 1))
    d["da_cb"] = np.ascontiguousarray(np.broadcast_to(tab[[15, 31]].T[None], (128, 8, 2)).astype(f32))
    d["ml_w_in"] = inp["ml_w_in"][0]
    d["ml_w_out"] = inp["ml_w_out"][0]
    mv = np.zeros((128, 16, 8), f32)
    for j in range(5):
        mv[:, :, j] = _fm(inp["ml_conv_w"][0][j], 16)
    mv[:, :, 5] = _fm(inp["ml_conv_b"][0], 16)
    mv[:, :, 6] = _fm(inp["ml_skip"][0], 16)
    mv[:, :, 7] = _fm(np.asarray(inp["ml_outnorm_g"][0]).reshape(-1), 16)
    d["ml_vec"] = mv
    bdm = np.zeros((128, 3, 16, 128), f32)
    pin = np.arange(128)
    for si, nm in enumerate(("ml_wq", "ml_wk", "ml_wv")):
        w = np.asarray(inp[nm][0], f32)
        for c in range(16):
            blk = w[c * 32:(c + 1) * 32]
            for o in range(4):
                bdm[pin, si, c, (pin // 4) * 4 + o] = blk[pin // 4, pin % 4, o]
    d["ml_bd"] = bdm
    gwt = np.asarray(inp["ml_gate_w"][0], f32)
    d["ml_gw"] = np.ascontiguousarray(gwt.reshape(2, 3, 16, 128, 8).transpose(3, 1, 2, 0, 4).reshape(128, 3, 16, 16))
    d["ml_gb"] = np.ascontiguousarray(np.broadcast_to(np.asarray(inp["ml_gate_b"][0], f32).reshape(1, 16), (128, 16)))
    d["moe_wr"] = np.ascontiguousarray(np.concatenate([inp["moe_w_group"], inp["moe_w_router"]], axis=2))
    d["moe_w1"] = inp["moe_w1"]
    d["moe_w3"] = inp["moe_w3"]
    d["moe_w2"] = inp["moe_w2"]
    return d


_NC_CACHE = {}


def kernel(**inputs):
    inp = {k: np.asarray(v) for k, v in inputs.items()}
    if "nc" not in _NC_CACHE:
        _NC_CACHE["nc"] = K().build()
    nc = _NC_CACHE["nc"]
    shared = None
    in_maps = []
    for core in range(8):
        d = host_layout(inp, core)
        if shared is None:
            shared = d
        else:
            for k in d:
                if k not in ("x", "cT"):
                    d[k] = shared[k]
        in_maps.append({k: np.ascontiguousarray(v, dtype=np.float32) for k, v in d.items()})
    res = run_bass_kernel_spmd(nc, in_maps, core_ids=list(range(8)))
    out = np.concatenate([r["out"] for r in res.results], axis=0)
    return out.astype(np.float32)
```
